# Optimizing a Trainium2 kernel written in Bass

```python
import math
import jax
import jax.numpy as jnp
from jax import lax
import numpy as np

D_MODEL = 2048
BATCH = 2
SEQ = 16384
DEPTH = 2

MEM_LEN = 256
HALF_MIX = D_MODEL // 2
MIX_WIDTH = 2 * HALF_MIX
IN_WIDTH = 5 * HALF_MIX
DIFF_HEADS = 4
DIFF_HEAD_DIM = HALF_MIX // (2 * DIFF_HEADS)
DIFF_V_DIM = 2 * DIFF_HEAD_DIM
ROPE_DIM = DIFF_HEAD_DIM // 4
ROPE_THETA = 500000.0
Q_BLOCK = 128
CONF_WIDTH = HALF_MIX
CONF_KERNEL = 31
SGU_WIDTH = HALF_MIX
SGU_GROUPS = 4
SGU_GROUP_DIM = SGU_WIDTH // SGU_GROUPS
SGU_CHUNK = 128
SC_WIDTH = HALF_MIX
SC_KERNEL = 3
CROSS_HEADS = 4
CROSS_HEAD_DIM = 128
CROSS_WIDTH = CROSS_HEADS * CROSS_HEAD_DIM
N_GROUPS = 4
EXPERTS_PER_GROUP = 8
N_EXPERTS = N_GROUPS * EXPERTS_PER_GROUP
TOP_K = 2
EXPERT_FF = D_MODEL // 4
MOE_BLOCK = 128
N_EVEN = (DEPTH + 1) // 2
N_ODD = DEPTH // 2
ALPHA = (2 * DEPTH) ** 0.25
BETA = (8 * DEPTH) ** -0.25
LN_EPS = 1e-5

kernel_name = 'hybrid_diffattn_conformer_sgu_shortconv_hmoe'


def layer_norm(x, g, b):
    xf = x.astype(jnp.float32)
    mu = jnp.mean(xf, axis=-1, keepdims=True)
    var = jnp.mean(jnp.square(xf - mu), axis=-1, keepdims=True)
    y = (xf - mu) * lax.rsqrt(var + LN_EPS) * g.astype(jnp.float32) + b.astype(jnp.float32)
    return y.astype(x.dtype)


def rms_norm(x, g):
    xf = x.astype(jnp.float32)
    y = xf * lax.rsqrt(jnp.mean(jnp.square(xf), axis=-1, keepdims=True) + LN_EPS) * g.astype(jnp.float32)
    return y.astype(x.dtype)


def rope_tables(positions, dtype):
    inv_freq = ROPE_THETA ** (-jnp.arange(0, ROPE_DIM, 2, dtype=jnp.float32) / ROPE_DIM)
    ang = positions.astype(jnp.float32)[..., None] * inv_freq
    return jnp.cos(ang)[:, :, None, :].astype(dtype), jnp.sin(ang)[:, :, None, :].astype(dtype)


def partial_rope(t, cos, sin):
    half = ROPE_DIM // 2
    t1 = t[..., :half]
    t2 = t[..., half:ROPE_DIM]
    return jnp.concatenate([t1 * cos - t2 * sin, t2 * cos + t1 * sin, t[..., ROPE_DIM:]], axis=-1)


def causal_depthwise_conv(x, w):
    k = w.shape[0]
    xp = jnp.pad(x, ((0, 0), (k - 1, 0), (0, 0)))
    return lax.conv_general_dilated(xp, w[:, None, :].astype(x.dtype), window_strides=(1,), padding='VALID',
                                    dimension_numbers=('NWC', 'WIO', 'NWC'), feature_group_count=x.shape[-1])


def diff_attention(q1, q2, k1, k2, v, lam):
    bsz, heads, seq, dh = q1.shape
    nb = seq // Q_BLOCK
    scale = dh ** -0.5
    kpos = jnp.arange(seq)

    def to_blocks(q):
        return q.reshape(bsz, heads, nb, Q_BLOCK, dh).transpose(2, 0, 1, 3, 4)

    def one_block(args):
        i, a1, a2 = args
        qpos = i * Q_BLOCK + jnp.arange(Q_BLOCK)
        mask = kpos[None, :] <= qpos[:, None]

        def attn_map(a, k):
            s = jnp.einsum('bhqd,bhkd->bhqk', a, k).astype(jnp.float32) * scale
            return jax.nn.softmax(jnp.where(mask, s, -jnp.inf), axis=-1)

        p = attn_map(a1, k1) - lam * attn_map(a2, k2)
        return jnp.einsum('bhqk,bhkd->bhqd', p.astype(v.dtype), v)

    out = lax.map(one_block, (jnp.arange(nb), to_blocks(q1), to_blocks(q2)))
    return out.transpose(1, 2, 0, 3, 4).reshape(bsz, heads, seq, v.shape[-1])


def even_mixer(h, cos, sin, layer_idx, lq1, lk1, lq2, lk2, subln_g, conv_w, conv_b, cln_g, cln_b):
    bsz, seq, _ = h.shape
    hd = DIFF_HEADS
    q = h[..., 0:HALF_MIX].reshape(bsz, seq, 2 * hd, DIFF_HEAD_DIM)
    k = h[..., HALF_MIX:2 * HALF_MIX].reshape(bsz, seq, 2 * hd, DIFF_HEAD_DIM)
    v = h[..., 2 * HALF_MIX:3 * HALF_MIX].reshape(bsz, seq, hd, DIFF_V_DIM).transpose(0, 2, 1, 3)
    glu_in = h[..., 3 * HALF_MIX:]

    def split_maps(t):
        t = partial_rope(t, cos, sin).transpose(0, 2, 1, 3).reshape(bsz, hd, 2, seq, DIFF_HEAD_DIM)
        return t[:, :, 0], t[:, :, 1]

    q1, q2 = split_maps(q)
    k1, k2 = split_maps(k)
    lam_init = 0.8 - 0.6 * math.exp(-0.3 * layer_idx)
    lam = (jnp.exp(jnp.sum(lq1.astype(jnp.float32) * lk1.astype(jnp.float32)))
           - jnp.exp(jnp.sum(lq2.astype(jnp.float32) * lk2.astype(jnp.float32))) + lam_init)
    o = diff_attention(q1, q2, k1, k2, v, lam)
    o = rms_norm(o, subln_g) * (1.0 - lam_init)
    o = o.transpose(0, 2, 1, 3).reshape(bsz, seq, hd * DIFF_V_DIM)
    a, g = jnp.split(glu_in, 2, axis=-1)
    c = a * jax.nn.sigmoid(g)
    c = causal_depthwise_conv(c, conv_w) + conv_b.astype(c.dtype)
    c = jax.nn.silu(layer_norm(c, cln_g, cln_b))
    return jnp.concatenate([o, c], axis=-1)


def odd_mixer(h, ln_g, ln_b, sgu_w, sgu_b, sc_w):
    bsz, seq, _ = h.shape
    nc = seq // SGU_CHUNK
    z = jax.nn.gelu(h[..., :2 * SGU_WIDTH], approximate=False)
    u, vg = jnp.split(z, 2, axis=-1)
    vg = layer_norm(vg, ln_g, ln_b).reshape(bsz, nc, SGU_CHUNK, SGU_GROUPS, SGU_GROUP_DIM)
    w_causal = sgu_w * jnp.tril(jnp.ones((SGU_CHUNK, SGU_CHUNK), sgu_w.dtype))
    sv = jnp.einsum('gts,bnsgc->bntgc', w_causal, vg) + sgu_b.T[:, :, None].astype(vg.dtype)
    spatial = u * sv.reshape(bsz, seq, SGU_WIDTH)
    gb, gc, xin = jnp.split(h[..., 2 * SGU_WIDTH:], 3, axis=-1)
    conv = gb * causal_depthwise_conv(gc * xin, sc_w)
    return jnp.concatenate([spatial, conv], axis=-1)


def memory_cross_attention(x, mem_k, mem_v, wq, wo):
    bsz, seq, _ = x.shape
    q = (x @ wq).reshape(bsz, seq, CROSS_HEADS, CROSS_HEAD_DIM)
    s = jnp.einsum('bshd,bmhd->bhsm', q, mem_k).astype(jnp.float32) * CROSS_HEAD_DIM ** -0.5
    p = jax.nn.softmax(s, axis=-1).astype(x.dtype)
    o = jnp.einsum('bhsm,bmhd->bshd', p, mem_v).reshape(bsz, seq, CROSS_WIDTH)
    return o @ wo


def hier_moe(x, rg_w, rg_b, re_w, re_b, w1, w3, w2):
    bsz, seq, d = x.shape
    xt = x.reshape(-1, d)
    n_tok = xt.shape[0]
    g_logits = (xt @ rg_w + rg_b).astype(jnp.float32)
    grp = jnp.argmax(g_logits, axis=-1)
    g_gate = jnp.take_along_axis(jax.nn.softmax(g_logits, axis=-1), grp[:, None], axis=1)[:, 0]
    e_logits = (xt @ re_w + re_b).astype(jnp.float32).reshape(n_tok, N_GROUPS, EXPERTS_PER_GROUP)
    e_logits = jnp.take_along_axis(e_logits, grp[:, None, None], axis=1)[:, 0]
    top_v, top_i = lax.top_k(e_logits, TOP_K)
    gate = jax.nn.softmax(top_v, axis=-1) * g_gate[:, None]
    eid = (grp[:, None] * EXPERTS_PER_GROUP + top_i).reshape(-1).astype(jnp.int32)
    n_assign = eid.shape[0]
    tok = jnp.repeat(jnp.arange(n_tok, dtype=jnp.int32), TOP_K)
    wgt = gate.reshape(-1)
    order = jnp.argsort(eid, stable=True)
    se, stok, sw = eid[order], tok[order], wgt[order]
    counts = jnp.bincount(eid, length=N_EXPERTS)
    padded = (counts + MOE_BLOCK - 1) // MOE_BLOCK * MOE_BLOCK
    start = jnp.cumsum(counts) - counts
    pend = jnp.cumsum(padded)
    pstart = pend - padded
    dest = pstart[se] + jnp.arange(n_assign, dtype=jnp.int32) - start[se]
    n_blocks = -(-n_assign // MOE_BLOCK) + N_EXPERTS
    buf_tok = jnp.zeros((n_blocks * MOE_BLOCK,), jnp.int32).at[dest].set(stok)
    buf_w = jnp.zeros((n_blocks * MOE_BLOCK,), jnp.float32).at[dest].set(sw)
    blk_e = jnp.minimum(jnp.searchsorted(pend, jnp.arange(n_blocks, dtype=jnp.int32) * MOE_BLOCK, side='right'),
                        N_EXPERTS - 1)

    def run(args):
        e, ti, wi = args
        xb = xt[ti]
        hb = jax.nn.silu(xb @ w1[e]) * (xb @ w3[e])
        return (hb @ w2[e]) * wi[:, None].astype(xb.dtype)

    y = lax.map(run, (blk_e, buf_tok.reshape(n_blocks, MOE_BLOCK), buf_w.reshape(n_blocks, MOE_BLOCK)))
    out = jax.ops.segment_sum(y.reshape(-1, d), buf_tok, num_segments=n_tok)
    return out.reshape(bsz, seq, d)


def setup_inputs(seed: int = 0) -> dict:
    key = jax.random.key(seed)
    ks = iter(jax.random.split(key, 40))

    def nrm(shape, scale):
        return jax.random.normal(next(ks), shape, jnp.float32) * scale

    L = DEPTH
    x = nrm((BATCH, SEQ, D_MODEL), 1.0)
    mem = nrm((BATCH, MEM_LEN, D_MODEL), 1.0)
    positions = (jax.random.randint(next(ks), (BATCH, 1), 0, 4096, jnp.int32)
                 + jnp.arange(SEQ, dtype=jnp.int32)[None, :])
    w_in = nrm((L, D_MODEL, IN_WIDTH), D_MODEL ** -0.5)
    w_in = w_in.at[0::2, :, 2 * HALF_MIX:3 * HALF_MIX].multiply(BETA)
    w_out = nrm((L, MIX_WIDTH, D_MODEL), BETA * MIX_WIDTH ** -0.5)
    ln_mix_g = 1.0 + nrm((L, D_MODEL), 0.02)
    ln_mix_b = nrm((L, D_MODEL), 0.02)
    ln_mem_g = 1.0 + nrm((L, D_MODEL), 0.02)
    ln_mem_b = nrm((L, D_MODEL), 0.02)
    ln_ffn_g = 1.0 + nrm((L, D_MODEL), 0.02)
    ln_ffn_b = nrm((L, D_MODEL), 0.02)
    lam_q1 = nrm((N_EVEN, DIFF_HEAD_DIM), 0.1)
    lam_k1 = nrm((N_EVEN, DIFF_HEAD_DIM), 0.1)
    lam_q2 = nrm((N_EVEN, DIFF_HEAD_DIM), 0.1)
    lam_k2 = nrm((N_EVEN, DIFF_HEAD_DIM), 0.1)
    diff_subln_g = 1.0 + nrm((N_EVEN, DIFF_V_DIM), 0.02)
    conv_w = nrm((N_EVEN, CONF_KERNEL, CONF_WIDTH), CONF_KERNEL ** -0.5)
    conv_b = nrm((N_EVEN, CONF_WIDTH), 0.02)
    conv_ln_g = 1.0 + nrm((N_EVEN, CONF_WIDTH), 0.02)
    conv_ln_b = nrm((N_EVEN, CONF_WIDTH), 0.02)
    sgu_ln_g = 1.0 + nrm((N_ODD, SGU_WIDTH), 0.02)
    sgu_ln_b = nrm((N_ODD, SGU_WIDTH), 0.02)
    sgu_w = nrm((N_ODD, SGU_GROUPS, SGU_CHUNK, SGU_CHUNK), SGU_CHUNK ** -0.5)
    sgu_b = 1.0 + nrm((N_ODD, SGU_GROUPS, SGU_CHUNK), 0.02)
    sc_w = nrm((N_ODD, SC_KERNEL, SC_WIDTH), SC_KERNEL ** -0.5)
    mem_kv_w = nrm((D_MODEL, 2 * CROSS_WIDTH), D_MODEL ** -0.5)
    mem_kv_w = mem_kv_w.at[:, CROSS_WIDTH:].multiply(BETA)
    xq_w = nrm((L, D_MODEL, CROSS_WIDTH), D_MODEL ** -0.5)
    xo_w = nrm((L, CROSS_WIDTH, D_MODEL), BETA * CROSS_WIDTH ** -0.5)
    rg_w = nrm((L, D_MODEL, N_GROUPS), D_MODEL ** -0.5)
    rg_b = nrm((L, N_GROUPS), 0.01)
    re_w = nrm((L, D_MODEL, N_EXPERTS), D_MODEL ** -0.5)
    re_b = nrm((L, N_EXPERTS), 0.01)
    e_w1 = nrm((L, N_EXPERTS, D_MODEL, EXPERT_FF), D_MODEL ** -0.5)
    e_w3 = nrm((L, N_EXPERTS, D_MODEL, EXPERT_FF), D_MODEL ** -0.5)
    e_w2 = nrm((L, N_EXPERTS, EXPERT_FF, D_MODEL), BETA * EXPERT_FF ** -0.5)
    return {'x': x, 'mem': mem, 'positions': positions, 'w_in': w_in, 'w_out': w_out,
            'ln_mix_g': ln_mix_g, 'ln_mix_b': ln_mix_b, 'ln_mem_g': ln_mem_g, 'ln_mem_b': ln_mem_b,
            'ln_ffn_g': ln_ffn_g, 'ln_ffn_b': ln_ffn_b,
            'lam_q1': lam_q1, 'lam_k1': lam_k1, 'lam_q2': lam_q2, 'lam_k2': lam_k2, 'diff_subln_g': diff_subln_g,
            'conv_w': conv_w, 'conv_b': conv_b, 'conv_ln_g': conv_ln_g, 'conv_ln_b': conv_ln_b,
            'sgu_ln_g': sgu_ln_g, 'sgu_ln_b': sgu_ln_b, 'sgu_w': sgu_w, 'sgu_b': sgu_b, 'sc_w': sc_w,
            'mem_kv_w': mem_kv_w, 'xq_w': xq_w, 'xo_w': xo_w,
            'rg_w': rg_w, 'rg_b': rg_b, 're_w': re_w, 're_b': re_b, 'e_w1': e_w1, 'e_w3': e_w3, 'e_w2': e_w2}


def reference(x, mem, positions, w_in, w_out, ln_mix_g, ln_mix_b, ln_mem_g, ln_mem_b, ln_ffn_g, ln_ffn_b,
              lam_q1, lam_k1, lam_q2, lam_k2, diff_subln_g, conv_w, conv_b, conv_ln_g, conv_ln_b,
              sgu_ln_g, sgu_ln_b, sgu_w, sgu_b, sc_w, mem_kv_w, xq_w, xo_w,
              rg_w, rg_b, re_w, re_b, e_w1, e_w3, e_w2):
    bsz = x.shape[0]
    n_mem = mem.shape[1]
    cos, sin = rope_tables(positions, x.dtype)
    kv = (mem @ mem_kv_w).reshape(bsz, n_mem, 2, CROSS_HEADS, CROSS_HEAD_DIM)
    mem_k, mem_v = kv[:, :, 0], kv[:, :, 1]
    for l in range(DEPTH):
        h = x @ w_in[l]
        j = l // 2
        if l % 2 == 0:
            mix = even_mixer(h, cos, sin, l, lam_q1[j], lam_k1[j], lam_q2[j], lam_k2[j], diff_subln_g[j],
                             conv_w[j], conv_b[j], conv_ln_g[j], conv_ln_b[j])
        else:
            mix = odd_mixer(h, sgu_ln_g[j], sgu_ln_b[j], sgu_w[j], sgu_b[j], sc_w[j])
        x = layer_norm(ALPHA * x + mix @ w_out[l], ln_mix_g[l], ln_mix_b[l])
        x = layer_norm(ALPHA * x + memory_cross_attention(x, mem_k, mem_v, xq_w[l], xo_w[l]),
                       ln_mem_g[l], ln_mem_b[l])
        x = layer_norm(ALPHA * x + hier_moe(x, rg_w[l], rg_b[l], re_w[l], re_b[l], e_w1[l], e_w3[l], e_w2[l]),
                       ln_ffn_g[l], ln_ffn_b[l])
    return x
```

```python
import numpy as np
import concourse.bass as bass
import concourse.mybir as mybir
from concourse.bass_utils import run_bass_kernel_spmd
from contextlib import ExitStack, contextmanager

F32 = mybir.dt.float32
BF16 = mybir.dt.bfloat16
I32 = mybir.dt.int32
ALU = mybir.AluOpType
AF = mybir.ActivationFunctionType
AX = mybir.AxisListType

COMPUTE = ("tensor", "vector", "scalar", "gpsimd")
ENGS = ("tensor", "vector", "scalar", "gpsimd", "sync")
NDSEM = 6
SB_LIMIT = 228000


class Op:
    __slots__ = ("eng", "fn", "reads", "writes", "dma", "deps", "signal", "seq", "didx", "waits")

    def __init__(self, eng, fn, reads, writes, dma):
        self.eng = eng
        self.fn = fn
        self.reads = tuple(reads)
        self.writes = tuple(writes)
        self.dma = dma
        self.deps = set()
        self.signal = False
        self.seq = 0
        self.didx = -1
        self.waits = []


class Prog:
    def __init__(self, nc):
        self.nc = nc
        self.ops = []
        self.stack = ExitStack()
        self.npsum = 0
        self.sb_off = 16384
        self.sb_peak = 0
        self.nalloc = 0

    def sb(self, name, shape, dt):
        esz = {F32: 4, BF16: 2, I32: 4}[dt]
        n = 1
        for d in shape[1:]:
            n *= d
        nbytes = (n * esz + 63) // 64 * 64
        off = self.sb_off
        assert off + nbytes <= SB_LIMIT, ("SBUF overflow", name, off, nbytes)
        self.sb_off = off + nbytes
        self.sb_peak = max(self.sb_peak, self.sb_off)
        self.nalloc += 1
        return self.nc.alloc_sbuf_tensor_at("%s_%d" % (name, self.nalloc), list(shape), dt, offset=off)

    @contextmanager
    def scope(self):
        self.barrier()
        save = self.sb_off
        try:
            yield
        finally:
            self.sb_off = save
            self.barrier()

    def ps(self, name, shape, dt=F32):
        return self.stack.enter_context(self.nc.psum_tensor(name, list(shape), dt))

    def op(self, eng, fn, reads=(), writes=(), dma=False):
        o = Op(eng, fn, reads, writes, dma)
        self.ops.append(o)
        return o

    def dma(self, eng, out, in_, reads=(), writes=(), **kw):
        return self.op(eng, lambda e: e.dma_start(out=out, in_=in_, **kw), reads, writes, dma=True)

    def mm(self, out, lhsT, rhs, start=True, stop=True, reads=(), writes=(), **kw):
        return self.op("tensor", lambda e: e.matmul(out, lhsT, rhs, start=start, stop=stop, **kw), reads, writes)

    def tr(self, out, in_, ident, reads=(), writes=()):
        return self.op("tensor", lambda e: e.transpose(out, in_, ident), reads, writes)

    def act(self, out, in_, func, reads=(), writes=(), **kw):
        return self.op("scalar", lambda e: e.activation(out, in_, func, **kw), reads, writes)

    def barrier(self):
        self.ops.append(Op(None, None, (), (), False))

    def tt(self, eng, out, in0, in1, op, reads=(), writes=()):
        return self.op(eng, lambda e: e.tensor_tensor(out, in0, in1, op), reads, writes)

    def ts(self, eng, out, in0, s1, s2, op0, op1=None, reads=(), writes=(), **kw):
        if op1 is None:
            return self.op(eng, lambda e: e.tensor_scalar(out, in0, s1, s2, op0, **kw), reads, writes)
        return self.op(eng, lambda e: e.tensor_scalar(out, in0, s1, s2, op0, op1, **kw), reads, writes)

    def stt(self, eng, out, in0, scalar, in1, op0, op1, reads=(), writes=()):
        return self.op(eng, lambda e: e.scalar_tensor_tensor(out, in0, scalar, in1, op0, op1), reads, writes)

    def copy(self, eng, out, in_, reads=(), writes=()):
        if eng == "scalar":
            return self.op(eng, lambda e: e.copy(out, in_), reads, writes)
        return self.op(eng, lambda e: e.tensor_copy(out, in_), reads, writes)

    def memset(self, eng, ap, val, writes=()):
        return self.op(eng, lambda e: e.memset(ap, val), (), writes)

    def recip(self, out, in_, reads=(), writes=()):
        return self.op("vector", lambda e: e.reciprocal(out, in_), reads, writes)

    def red(self, eng, out, in_, op, reads=(), writes=()):
        return self.op(eng, lambda e: e.tensor_reduce(out, in_, AX.X, op), reads, writes)

    def build(self):
        nc = self.nc
        ops = self.ops
        last_w = {}
        readers = {}
        pend = {}
        lastc = {}
        lastd = {e: [] for e in ENGS}
        for i, o in enumerate(ops):
            if o.eng is None:
                bd = set(lastc.values())
                for e in ENGS:
                    bd.update(lastd[e][-NDSEM:])
                for e in ENGS:
                    pend[e] = set(bd) | pend.get(e, set())
                continue
            deps = set(pend.pop(o.eng, ()))
            if o.dma:
                lastd[o.eng].append(i)
            else:
                lastc[o.eng] = i
            for k in o.reads:
                w = last_w.get(k)
                if w is not None:
                    deps.add(w)
            for k in o.writes:
                w = last_w.get(k)
                if w is not None:
                    deps.add(w)
                deps.update(readers.get(k, ()))
            deps.discard(i)
            for k in o.writes:
                last_w[k] = i
                readers[k] = []
            for k in o.reads:
                readers.setdefault(k, []).append(i)
            o.deps = {d for d in deps if not (o.eng == "tensor" and ops[d].eng == "tensor" and not o.dma and not ops[d].dma)}
            for d in o.deps:
                ops[d].signal = True
        st = self.stack
        csem = {e: st.enter_context(nc.semaphore("c_" + e)) for e in COMPUTE}
        dsem = {e: [st.enter_context(nc.semaphore("d_%s%d" % (e, j))) for j in range(NDSEM)] for e in ENGS}
        cseq = {e: 0 for e in COMPUTE}
        dcnt = {e: 0 for e in ENGS}
        ops_all = ops
        ops = [o for o in ops_all if o.eng is not None]
        for o in ops:
            if o.dma:
                o.didx = dcnt[o.eng]
                dcnt[o.eng] += 1
            elif o.signal:
                cseq[o.eng] += 1
                o.seq = cseq[o.eng]

        def done_cond(d):
            od = ops_all[d]
            if od.dma:
                return (dsem[od.eng][od.didx % NDSEM], 16 * (od.didx // NDSEM + 1))
            return (csem[od.eng], od.seq)

        waited = {e: {} for e in ENGS}
        for o in ops:
            conds = {}
            if o.dma and o.didx >= NDSEM:
                s = dsem[o.eng][o.didx % NDSEM]
                conds[id(s)] = (s, 16 * (o.didx // NDSEM))
            for d in o.deps:
                s, v = done_cond(d)
                if id(s) not in conds or conds[id(s)][1] < v:
                    conds[id(s)] = (s, v)
            wl = waited[o.eng]
            for sid, (s, v) in conds.items():
                if wl.get(sid, 0) < v:
                    wl[sid] = v
                    o.waits.append((s, v))
        final_waits = []
        for e in ENGS:
            n = dcnt[e]
            for j in range(NDSEM):
                cnt = (n - j + NDSEM - 1) // NDSEM if n > j else 0
                if cnt > 0:
                    final_waits.append((dsem[e][j], 16 * cnt))
        byeng = {e: [o for o in ops if o.eng == e] for e in ENGS}
        self.n_inst = {e: len(byeng[e]) for e in ENGS}

        def emit(eng_name):
            def f(eng):
                for o in byeng[eng_name]:
                    for s, v in o.waits:
                        eng.wait_ge(s, v)
                    ins = o.fn(eng)
                    if o.dma:
                        ins.then_inc(dsem[o.eng][o.didx % NDSEM], 16)
                    elif o.signal:
                        ins.then_inc(csem[o.eng], 1)
                if eng_name == "sync":
                    for s, v in final_waits:
                        eng.wait_ge(s, v)
            return f

        with nc.Block() as block:
            block.sync(emit("sync"))
            block.tensor(emit("tensor"))
            block.vector(emit("vector"))
            block.scalar(emit("scalar"))
            block.gpsimd(emit("gpsimd"))
        self.stack.close()
        return nc


D = 2048
ALPHA = float(4 ** 0.25)
EPS = 1e-5
NEG_BIG = -1.0e30
CAP = 512


class Rot:
    def __init__(self, P, name, shape, dt, n, psum=False):
        self.i = 0
        self.name = name
        if psum:
            if not hasattr(P, "banks"):
                P.banks = [P.ps("bank%d" % i, [128, 512], F32) for i in range(8)]
                P.bank_next = 0
            idx = [(P.bank_next + i) % 8 for i in range(n)]
            P.bank_next = (P.bank_next + n) % 8
            if dt == BF16:
                self.t = [P.banks[i][:, :].bitcast(BF16) for i in idx]
            else:
                self.t = [P.banks[i] for i in idx]
            self.keys = [("bank", i) for i in idx]
        else:
            self.t = [P.sb("%s%d" % (name, i), shape, dt) for i in range(n)]
            self.keys = [(name, j) for j in range(n)]

    def next(self):
        j = self.i % len(self.t)
        self.i += 1
        return self.t[j], self.keys[j]


def new_nc():
    return bass.Bass("TRN2", target_bir_lowering=False)


def din(nc, name, shape, dt=F32):
    return nc.dram_tensor(name, list(shape), dt, kind="ExternalInput").ap()


def dout(nc, name, shape, dt=F32):
    return nc.dram_tensor(name, list(shape), dt, kind="ExternalOutput").ap()


def dscr(nc, name, shape, dt):
    return nc.dram_tensor(name, list(shape), dt, kind="Internal").ap()


def setup_consts(P):
    C = {}
    idf = P.sb("ident_f", [128, 128], F32)
    P.memset("gpsimd", idf[:], 0.0, writes=["ident_f"])
    P.op("gpsimd", lambda e: e.affine_select(idf[:], idf[:], [[-1, 128]], ALU.not_equal, 1.0, base=0, channel_multiplier=1),
         reads=["ident_f"], writes=["ident_f"])
    idb = P.sb("ident_b", [128, 128], BF16)
    P.copy("vector", idb[:], idf[:], reads=["ident_f"], writes=["ident_b"])
    onf = P.sb("ones_f", [128, 128], F32)
    P.memset("vector", onf[:], 1.0, writes=["ones_f"])
    onb = P.sb("ones_b", [128, 128], BF16)
    P.memset("vector", onb[:], 1.0, writes=["ones_b"])
    eps = P.sb("eps", [128, 1], F32)
    P.memset("vector", eps[:], EPS, writes=["eps"])
    C.update(ident_f=idf, ident_b=idb, ones_f=onf, ones_b=onb, eps=eps)
    return C


def load_bcast(P, name, src_row, W):
    t = P.sb(name, [128, W], F32)
    P.dma("sync", t[:], src_row.partition_broadcast(128), writes=[name])
    return t


def load_w_bf(P, name, w, K, N, eng="gpsimd"):
    kc = K // 128
    t = P.sb(name, [128, kc, N], BF16)
    wv = w.rearrange("(kc p) n -> p kc n", p=128)
    if not hasattr(P, "wstg"):
        P.wstg = Rot(P, "wstg", [128, 2048], F32, 3)
        P.wcnt = 0
    keys = []
    ces = ["scalar", "vector", "scalar", "gpsimd"]
    for c0 in range(0, N, 512):
        cw = min(512, N - c0)
        kstep = max(1, min(kc, 2048 // cw))
        for k0 in range(0, kc, kstep):
            kn = min(kstep, kc - k0)
            stg, stgk = P.wstg.next()
            sv = stg[:, 0:kn * cw].rearrange("p (k n) -> p k n", n=cw)
            P.dma("sync" if P.wcnt % 2 == 0 else "scalar", sv, wv[:, k0:k0 + kn, c0:c0 + cw], writes=[stgk])
            P.copy(ces[P.wcnt % len(ces)], t[:, k0:k0 + kn, c0:c0 + cw], sv, reads=[stgk], writes=[(name, c0, k0)])
            P.wcnt += 1
            keys.append((name, c0, k0))
    return t, keys


def load_w_bf_sw(P, name, w, K, N):
    kc = K // 128
    t = P.sb(name, [128, kc, N], BF16)
    wv = w.rearrange("(kc p) n -> p kc n", p=128)
    step = max(1, kc // 4)
    for k0 in range(0, kc, step):
        P.dma("gpsimd", t[:, k0:k0 + step, :], wv[:, k0:k0 + step, :], writes=[(name, k0)])
    return t, [(name, k0) for k0 in range(0, kc, step)]


class LN:
    def __init__(self, P, C, W, g_row, b_row, name):
        self.P, self.C, self.W, self.name = P, C, W, name
        self.gt = load_bcast(P, name + "_g", g_row, W)
        self.bt = load_bcast(P, name + "_b", b_row, W)
        self.nch = W // 512
        self.st = P.sb(name + "_st", [128, self.nch, 6], F32)
        self.mv = P.sb(name + "_mv", [128, 2], F32)
        self.rs = P.sb(name + "_rs", [128, 1], F32)


def _ln_emit_multi(P, ln, z, zkeys):
    n = ln.name
    for c in range(ln.nch):
        P.op("vector", lambda e, c=c: e.bn_stats(ln.st[:, c, :], z[:, c * 512:(c + 1) * 512]),
             reads=list(zkeys), writes=[(n, "st", c)])
    P.op("vector", lambda e: e.bn_aggr(ln.mv[:, 0:2], ln.st[:].rearrange("p c s -> p (c s)")),
         reads=[(n, "st", c) for c in range(ln.nch)], writes=[(n, "mv")])
    P.act(ln.rs[:], ln.mv[:, 1:2], AF.Sqrt, bias=ln.C["eps"][:], scale=1.0, reads=[(n, "mv"), "eps"], writes=[(n, "rs")])
    P.recip(ln.rs[:], ln.rs[:], reads=[(n, "rs")], writes=[(n, "rs")])
    P.ts("vector", z[:], z[:], ln.mv[:, 0:1], ln.rs[:, 0:1], ALU.subtract, ALU.mult,
         reads=list(zkeys) + [(n, "mv"), (n, "rs")], writes=list(zkeys))
    P.tt("gpsimd", z[:], z[:], ln.gt[:], ALU.mult, reads=list(zkeys) + [n + "_g"], writes=list(zkeys))
    P.tt("gpsimd", z[:], z[:], ln.bt[:], ALU.add, reads=list(zkeys) + [n + "_b"], writes=list(zkeys))


def build_outproj(T):
    nc = new_nc()
    mixT = din(nc, "mixT", [D, T], BF16)
    xres = din(nc, "xres", [T, D])
    w = din(nc, "w", [D, D])
    g = din(nc, "g", [1, D])
    b = din(nc, "b", [1, D])
    y = dout(nc, "y", [T, D])
    yb = dout(nc, "yb", [T, D], BF16)
    P = Prog(nc)
    C = setup_consts(P)
    ln = LN(P, C, D, g, b, "ln")
    zbr = Rot(P, "zb", [128, D], BF16, 2)
    w_bf, wk = load_w_bf(P, "w_bf", w, D, D)
    mr = Rot(P, "mix", [128, 16, 512], BF16, 2)
    psr = Rot(P, "ps", [128, 512], F32, 4, psum=True)
    zr = Rot(P, "z", [128, D], F32, 2)
    xr = Rot(P, "xr", [128, D], F32, 2)
    mv = mixT.rearrange("(kc p) t -> p kc t", p=128)
    for tc in range(T // 512):
        m, mk = mr.next()
        P.dma("sync", m[:], mv[:, :, tc * 512:(tc + 1) * 512], writes=[mk])
        for tt in range(4):
            r0 = tc * 512 + tt * 128
            emit_proj_res_ln_multi(P, ln, m, [mk], tt, 16, w_bf, wk, xres[r0:r0 + 128, :], y[r0:r0 + 128, :], psr, zr, xr, (tc, tt), yb[r0:r0 + 128, :], zbr)
    P.build()
    return nc


def build_cross(T):
    nc = new_nc()
    xT = din(nc, "xT", [D, T], BF16)
    xres = din(nc, "xres", [T, D])
    memT = din(nc, "memT", [D, 256])
    kvw = din(nc, "kvw", [D, 1024])
    xqw = din(nc, "xqw", [D, 512])
    xow = din(nc, "xow", [512, D])
    g = din(nc, "g", [1, D])
    b = din(nc, "b", [1, D])
    y = dout(nc, "y", [T, D])
    yb = dout(nc, "yb", [T, D], BF16)
    P = Prog(nc)
    C = setup_consts(P)
    ln = LN(P, C, D, g, b, "ln")
    zbr = Rot(P, "zb", [128, D], BF16, 2)
    xq_bf, xqk = load_w_bf(P, "xq_bf", xqw, D, 512)
    xo_bf, xok = load_w_bf(P, "xo_bf", xow, 512, D)
    psr = Rot(P, "ps", [128, 512], F32, 4, psum=True)
    pso = Rot(P, "pso", [128, 512], F32, 2, psum=True)
    psl = Rot(P, "psl", [128, 512], F32, 2, psum=True)
    kmT = P.sb("kmT", [128, 4, 256], BF16)
    vm = P.sb("vm", [128, 2, 512], BF16)
    with P.scope():
        kvw_bf, kvk = load_w_bf(P, "kvw_bf", kvw, D, 1024)
        memT_bf, mk_ = load_w_bf(P, "memT_bf", memT, D, 256)
        for h in range(4):
            ps, pk = psr.next()
            for kc in range(16):
                P.mm(ps[:, 0:256], kvw_bf[:, kc, h * 128:(h + 1) * 128], memT_bf[:, kc, :], start=(kc == 0), stop=(kc == 15),
                     reads=kvk + mk_, writes=[pk])
            P.copy("vector", kmT[:, h, :], ps[:, 0:256], reads=[pk], writes=[("kmT", h)])
        for mt in range(2):
            ps, pk = psr.next()
            for kc in range(16):
                P.mm(ps[:, :], memT_bf[:, kc, mt * 128:(mt + 1) * 128], kvw_bf[:, kc, 512:1024], start=(kc == 0), stop=(kc == 15),
                     reads=kvk + mk_, writes=[pk])
            P.copy("vector", vm[:, mt, :], ps[:, :], reads=[pk], writes=[("vm", mt)])
    kmk = [("kmT", h) for h in range(4)]
    vmk = [("vm", mt) for mt in range(2)]
    xr_ = Rot(P, "xTc", [128, 16, 512], BF16, 2)
    qr = Rot(P, "qT", [128, 512], BF16, 2)
    pr = Rot(P, "pT", [128, 512], BF16, 4)
    rlr = Rot(P, "rL", [128, 512], F32, 2)
    otr = Rot(P, "oT", [128, 4, 512], BF16, 2)
    zr = Rot(P, "z", [128, D], F32, 2)
    xr = Rot(P, "xr", [128, D], F32, 2)
    xv = xT.rearrange("(kc p) t -> p kc t", p=128)
    scale = float(128 ** -0.5)
    for tc in range(T // 512):
        xc, xk = xr_.next()
        for q4 in range(4):
            P.dma("sync", xc[:, q4 * 4:(q4 + 1) * 4, :], xv[:, q4 * 4:(q4 + 1) * 4, tc * 512:(tc + 1) * 512], writes=[(xk, q4)])
        xks = [(xk, q4) for q4 in range(4)]
        oT, ok = otr.next()
        for h in range(4):
            ps, pk = psr.next()
            for kc in range(16):
                P.mm(ps[:, :], xq_bf[:, kc, h * 128:(h + 1) * 128], xc[:, kc, :], start=(kc == 0), stop=(kc == 15),
                     reads=xqk + xks, writes=[pk])
            q, qk = qr.next()
            P.copy("scalar", q[:], ps[:, :], reads=[pk], writes=[qk])
            pts = []
            for mt in range(2):
                ps2, pk2 = psr.next()
                P.mm(ps2[:, :], kmT[:, h, mt * 128:(mt + 1) * 128], q[:], reads=kmk + [qk], writes=[pk2])
                pt, ptk = pr.next()
                P.act(pt[:], ps2[:, :], AF.Exp, scale=scale, reads=[pk2], writes=[ptk])
                pts.append((pt, ptk))
            po, pok = pso.next()
            pl, plk = psl.next()
            for mt in range(2):
                P.mm(po[:, :], vm[:, mt, h * 128:(h + 1) * 128], pts[mt][0][:], start=(mt == 0), stop=(mt == 1),
                     reads=vmk + [pts[mt][1]], writes=[pok])
            for mt in range(2):
                P.mm(pl[:, :], C["ones_b"][:], pts[mt][0][:], start=(mt == 0), stop=(mt == 1),
                     reads=["ones_b", pts[mt][1]], writes=[plk])
            rl, rlk = rlr.next()
            P.recip(rl[:], pl[:, :], reads=[plk], writes=[rlk])
            P.tt("vector", oT[:, h, :], po[:, :], rl[:], ALU.mult, reads=[pok, rlk], writes=[(ok, h)])
        oks = [(ok, h) for h in range(4)]
        for tt in range(4):
            r0 = tc * 512 + tt * 128
            emit_proj_res_ln_multi(P, ln, oT, oks, tt, 4, xo_bf, xok, xres[r0:r0 + 128, :], y[r0:r0 + 128, :], psr, zr, xr, (tc, tt), yb[r0:r0 + 128, :], zbr)
    P.build()
    return nc


def emit_proj_res_ln_multi(P, ln, lhs, lhsks, tt, KC, w_bf, wkeys, xres_ap, out_ap, psr, zr, xr, ntag, outb_ap=None, zbr=None):
    xt, xk = xr.next()
    P.dma("sync", xt[:], xres_ap, writes=[xk])
    z, zk = zr.next()
    for cg in range(4):
        ps, pk = psr.next()
        for kc in range(KC):
            P.mm(ps[:, :], lhs[:, kc, tt * 128:(tt + 1) * 128], w_bf[:, kc, cg * 512:(cg + 1) * 512],
                 start=(kc == 0), stop=(kc == KC - 1), reads=list(lhsks) + list(wkeys), writes=[pk])
        P.stt("vector", z[:, cg * 512:(cg + 1) * 512], xt[:, cg * 512:(cg + 1) * 512], ALPHA, ps[:, :], ALU.mult, ALU.add,
              reads=[xk, pk], writes=[(zk, cg)])
    zks = [(zk, cg) for cg in range(4)]
    _ln_emit_multi(P, ln, z, zks)
    P.dma("sync", out_ap, z[:], reads=zks, writes=[("out", ntag)])
    if outb_ap is not None:
        zb, zbk = zbr.next()
        P.copy("scalar", zb[:], z[:], reads=zks, writes=[zbk])
        P.dma("sync", outb_ap, zb[:], reads=[zbk], writes=[("outb", ntag)])


def build_moe(T, debug=False):
    NT = T // 128
    NS = CAP // 128
    nc = new_nc()
    xT = din(nc, "xT", [D, T])
    xres = din(nc, "xres", [T, D])
    rw = din(nc, "rw", [D, 36])
    rb = din(nc, "rb", [1, 36])
    ecap = din(nc, "ecap", [1, 32])
    w1 = din(nc, "w1", [32, D, 512])
    w3 = din(nc, "w3", [32, D, 512])
    w2 = din(nc, "w2", [32, 512, D])
    g = din(nc, "g", [1, D])
    b = din(nc, "b", [1, D])
    y = dout(nc, "y", [T, D])
    yb = dout(nc, "yb", [T, D], BF16)
    cnt_o = dout(nc, "cnt", [1, 32], F32)
    Xg = dscr(nc, "Xg", [32 * CAP, D], BF16)
    Yg = dscr(nc, "Yg", [32 * CAP, D], F32)
    P = Prog(nc)
    C = setup_consts(P)
    _bc = {}

    def _bcreg(e):
        if "r" not in _bc:
            _bc["r"] = e.to_reg(32 * CAP - 1)
        return _bc["r"]
    dest_i = P.sb("dest_i", [128, NT, 2], I32)
    gate = P.sb("gate", [128, NT, 2], F32)
    uf = P.sb("uf", [128, 128], F32)
    P.memset("gpsimd", uf[:], 1.0, writes=["uf"])
    P.op("gpsimd", lambda e: e.affine_select(uf[:], uf[:], [[1, 128]], ALU.is_ge, 0.0, base=-1, channel_multiplier=-1),
         reads=["uf"], writes=["uf"])
    ub = P.sb("ub", [128, 128], BF16)
    P.copy("vector", ub[:], uf[:], reads=["uf"], writes=["ub"])

    with P.scope():
        zt = P.sb("zt", [128, 8192], BF16)
        P.memset("gpsimd", zt[:], 0.0, writes=["zt"])
        xgz = Xg.rearrange("(n p f) d -> n p (f d)", p=128, f=4)
        assert (32 * CAP) % 512 == 0
        zkeys = []
        for i in range(32 * CAP // 512):
            P.dma("sync", xgz[i], zt[:], reads=["zt"], writes=[("xg0", i)])
            zkeys.append(("xg0", i))
        rw_f = P.sb("rw_f", [128, 16, 36], F32)
        P.dma("sync", rw_f[:], rw.rearrange("(kc p) n -> p kc n", p=128), writes=["rw_f"])
        rbt = load_bcast(P, "rbt", rb, 36)
        ect = load_bcast(P, "ect", ecap, 32)
        base = P.sb("base", [128, 32], F32)
        P.memset("vector", base[:], 0.0, writes=["base"])
        xcr = Rot(P, "xTf", [128, 16, 512], F32, 2)
        xrr = Rot(P, "xrt", [128, D], F32, 2)
        xbr = Rot(P, "xb", [128, D], BF16, 2)
        psr = Rot(P, "psr", [128, 512], F32, 2, psum=True)
        psq = Rot(P, "psq", [128, 512], F32, 2, psum=True)
        sm = {}
        for nm, w_, dt_ in [("lg", 36, F32), ("gmax", 1, F32), ("ngmax", 1, F32), ("gsel", 4, F32), ("gexp", 4, F32), ("gsum", 1, F32),
                            ("ggate", 1, F32), ("pen", 4, F32), ("em", 32, F32), ("top8", 8, F32), ("sel1", 32, F32), ("sel2", 32, F32),
                            ("dd", 1, F32), ("ee", 1, F32), ("den", 1, F32), ("p1", 1, F32), ("p2", 1, F32), ("A", 32, BF16),
                            ("slot", 32, F32), ("tmp", 32, F32), ("destf", 2, F32)]:
            sm[nm] = P.sb("r_" + nm, [128, w_], dt_)
        xv = xT.rearrange("(kc p) t -> p kc t", p=128)
        for tc in range(T // 512):
            xc, xk = xcr.next()
            for q4 in range(4):
                P.dma("sync", xc[:, q4 * 4:(q4 + 1) * 4, :], xv[:, q4 * 4:(q4 + 1) * 4, tc * 512:(tc + 1) * 512], writes=[(xk, q4)])
            xks = [(xk, q4) for q4 in range(4)]
            for tt in range(4):
                ti = tc * 4 + tt
                r0 = ti * 128
                ps, pk = psr.next()
                for kc in range(16):
                    P.mm(ps[:, 0:36], xc[:, kc, tt * 128:(tt + 1) * 128], rw_f[:, kc, :], start=(kc == 0), stop=(kc == 15),
                         reads=xks + ["rw_f"], writes=[pk])
                V = "vector"
                P.tt(V, sm["lg"][:], ps[:, 0:36], rbt[:], ALU.add, reads=[pk, "rbt"], writes=["lg"])
                P.red(V, sm["gmax"][:], sm["lg"][:, 0:4], ALU.max, reads=["lg"], writes=["gmax"])
                P.ts(V, sm["gsel"][:], sm["lg"][:, 0:4], sm["gmax"][:, 0:1], None, ALU.is_equal, reads=["lg", "gmax"], writes=["gsel"])
                P.ts(V, sm["ngmax"][:], sm["gmax"][:], -1.0, None, ALU.mult, reads=["gmax"], writes=["ngmax"])
                P.memset(V, sm["gsum"][:], 0.0, writes=["gsum"])
                P.act(sm["gexp"][:], sm["lg"][:, 0:4], AF.Exp, bias=sm["ngmax"][:], scale=1.0, accum_out=sm["gsum"][:],
                      reads=["lg", "ngmax", "gsum"], writes=["gexp", "gsum"])
                P.recip(sm["ggate"][:], sm["gsum"][:], reads=["gsum"], writes=["ggate"])
                P.ts(V, sm["pen"][:], sm["gsel"][:], -1.0, 1.0e30, ALU.add, ALU.mult, reads=["gsel"], writes=["pen"])
                for gi in range(4):
                    P.ts(V, sm["em"][:, gi * 8:(gi + 1) * 8], sm["lg"][:, 4 + gi * 8:4 + (gi + 1) * 8], sm["pen"][:, gi:gi + 1], None, ALU.add,
                         reads=["lg", "pen"], writes=[("em", gi)])
                emk = [("em", gi) for gi in range(4)]
                P.op(V, lambda e: e.max(sm["top8"][:], sm["em"][:]), reads=emk, writes=["top8"])
                P.ts(V, sm["sel1"][:], sm["em"][:], sm["top8"][:, 0:1], None, ALU.is_equal, reads=emk + ["top8"], writes=["sel1"])
                P.ts(V, sm["sel2"][:], sm["em"][:], sm["top8"][:, 1:2], None, ALU.is_equal, reads=emk + ["top8"], writes=["sel2"])
                P.tt(V, sm["dd"][:], sm["top8"][:, 1:2], sm["top8"][:, 0:1], ALU.subtract, reads=["top8"], writes=["dd"])
                P.act(sm["ee"][:], sm["dd"][:], AF.Exp, reads=["dd"], writes=["ee"])
                P.ts(V, sm["den"][:], sm["ee"][:], 1.0, None, ALU.add, reads=["ee"], writes=["den"])
                P.recip(sm["p1"][:], sm["den"][:], reads=["den"], writes=["p1"])
                P.tt(V, sm["p2"][:], sm["ee"][:], sm["p1"][:], ALU.mult, reads=["ee", "p1"], writes=["p2"])
                P.tt(V, gate[:, ti, 0:1], sm["p1"][:], sm["ggate"][:], ALU.mult, reads=["p1", "ggate"], writes=[("gate", ti, 0)])
                P.tt(V, gate[:, ti, 1:2], sm["p2"][:], sm["ggate"][:], ALU.mult, reads=["p2", "ggate"], writes=[("gate", ti, 1)])
                P.tt(V, sm["A"][:], sm["sel1"][:], sm["sel2"][:], ALU.add, reads=["sel1", "sel2"], writes=["A"])
                pq, pqk = psq.next()
                P.mm(pq[:, 0:32], ub[:], sm["A"][:], reads=["ub", "A"], writes=[(pqk, 0)])
                P.mm(pq[:, 32:64], C["ones_b"][:], sm["A"][:], reads=["ones_b", "A"], writes=[(pqk, 1)])
                P.tt(V, sm["slot"][:], pq[:, 0:32], base[:], ALU.add, reads=[(pqk, 0), "base"], writes=["slot"])
                P.tt(V, sm["slot"][:], sm["slot"][:], ect[:], ALU.add, reads=["slot", "ect"], writes=["slot"])
                P.tt(V, base[:], base[:], pq[:, 32:64], ALU.add, reads=["base", (pqk, 1)], writes=["base"])
                for k, sname in enumerate(("sel1", "sel2")):
                    P.tt(V, sm["tmp"][:], sm[sname][:], sm["slot"][:], ALU.mult, reads=[sname, "slot"], writes=["tmp"])
                    P.red(V, sm["destf"][:, k:k + 1], sm["tmp"][:], ALU.add, reads=["tmp"], writes=[("destf", k)])
                P.copy(V, dest_i[:, ti, :], sm["destf"][:], reads=[("destf", 0), ("destf", 1)], writes=[("dest", ti)])
                xr_t, xrk = xrr.next()
                P.dma("sync", xr_t[:], xres[r0:r0 + 128, :], writes=[xrk])
                xb, xbk = xbr.next()
                P.copy("scalar", xb[:], xr_t[:], reads=[xrk], writes=[xbk])
                for k in range(0 if debug == 1 else 2):
                    P.op("gpsimd", lambda e, xb=xb, ti=ti, k=k: e.indirect_dma_start(
                        out=Xg, out_offset=bass.IndirectOffsetOnAxis(ap=dest_i[:, ti, k:k + 1], axis=0), in_=xb[:], in_offset=None, bounds_check=_bcreg(e), oob_is_err=False),
                        reads=[xbk, ("dest", ti)] + zkeys, writes=[("xg", ti, k)], dma=True)
        P.dma("sync", cnt_o, base[0:1, :], reads=["base"], writes=["cnt_o"])
    xgkeys = [("xg", ti, k) for ti in range(NT) for k in range(2)]
    def dbg_exit():
        od = dout(nc, "o_dest", [128, NT, 2], I32)
        og = dout(nc, "o_gate", [128, NT, 2], F32)
        P.dma("sync", od, dest_i[:], reads=[("dest", ti) for ti in range(NT)])
        P.dma("sync", og, gate[:], reads=[("gate", ti, k) for ti in range(NT) for k in range(2)])
        P.barrier()
        P.build()
        return nc
    if debug in (1, 3):
        return dbg_exit()

    with P.scope():
        w1r = Rot(P, "w1b", [128, 16, 512], BF16, 2)
        w3r = Rot(P, "w3b", [128, 16, 512], BF16, 2)
        w2r = Rot(P, "w2b", [128, 4, D], BF16, 2)
        xgr = Rot(P, "xgr", [128, NS, D], BF16, 2)
        stgr = Rot(P, "wstg", [128, 2048], F32, 3)
        dcount = [0]
        xgT = P.sb("xgT", [128, 16, CAP], BF16)
        hhr = Rot(P, "hhT", [128, 4, CAP], BF16, 2)
        slr = Rot(P, "sl", [128, CAP], F32, 2)
        yrr = Rot(P, "yrow", [128, D], F32, 2)
        pst = Rot(P, "pst", [128, 512], BF16, 2, psum=True)
        psh = Rot(P, "psh", [128, 512], F32, 4, psum=True)
        psy = Rot(P, "psy", [128, 512], F32, 2, psum=True)
        ykeys = []
        cast_eng = ["gpsimd"]

        def load_expert(e_):
            w1b, w1k = w1r.next()
            w3b, w3k = w3r.next()
            w2b, w2k = w2r.next()
            w2v_ = w2[e_].rearrange("(kc p) n -> p kc n", p=128)
            for h2 in range(2):
                P.dma("gpsimd", w2b[:, h2 * 2:(h2 + 1) * 2, :], w2v_[:, h2 * 2:(h2 + 1) * 2, :], writes=[(w2k, h2)])
            for wi, (wsrc, wdst, wkk, npiece, kcs) in enumerate(((w1, w1b, w1k, 4, 4), (w3, w3b, w3k, 4, 4))):
                wv_ = wsrc[e_].rearrange("(kc p) n -> p kc n", p=128)
                for pc_ in range(npiece):
                    stg, stgk = stgr.next()
                    ncol = 512 if wi < 2 else D
                    sv = stg[:, :].rearrange("p (k n) -> p k n", n=ncol)
                    P.dma("sync", sv, wv_[:, pc_ * kcs:(pc_ + 1) * kcs, :], writes=[stgk])
                    ce = cast_eng[dcount[0] % len(cast_eng)]
                    dcount[0] += 1
                    P.copy(ce, wdst[:, pc_ * kcs:(pc_ + 1) * kcs, :], sv, reads=[stgk], writes=[(wkk, pc_)])
            xg, xgk = xgr.next()
            P.dma("sync", xg[:], Xg[e_ * CAP:(e_ + 1) * CAP, :].rearrange("(s p) d -> p s d", p=128), reads=xgkeys, writes=[xgk])
            return (w1b, [(w1k, q_) for q_ in range(4)], w3b, [(w3k, q_) for q_ in range(4)], w2b, [(w2k, q_) for q_ in range(2)], xg, xgk)

        def compute_expert(e_, ld):
            w1b, w1ks, w3b, w3ks, w2b, w2ks, xg, xgk = ld
            for kc in range(16):
                pt, ptk = pst.next()
                for s_ in range(NS):
                    P.tr(pt[:, s_ * 128:(s_ + 1) * 128], xg[:, s_, kc * 128:(kc + 1) * 128], C["ident_b"][:], reads=[xgk, "ident_b"], writes=[ptk])
                P.copy("vector" if kc % 2 == 0 else "scalar", xgT[:, kc, :], pt[:, 0:CAP], reads=[ptk], writes=[("xgT", kc)])
            xtk = [("xgT", kc) for kc in range(16)]
            hh, hhk = hhr.next()
            for fc in range(4):
                p1_, p1k = psh.next()
                for kc in range(16):
                    P.mm(p1_[:, 0:CAP], w1b[:, kc, fc * 128:(fc + 1) * 128], xgT[:, kc, :], start=(kc == 0), stop=(kc == 15), reads=w1ks + xtk, writes=[p1k])
                p3_, p3k = psh.next()
                for kc in range(16):
                    P.mm(p3_[:, 0:CAP], w3b[:, kc, fc * 128:(fc + 1) * 128], xgT[:, kc, :], start=(kc == 0), stop=(kc == 15), reads=w3ks + xtk, writes=[p3k])
                sl, slk = slr.next()
                P.act(sl[:], p1_[:, 0:CAP], AF.Silu, reads=[p1k], writes=[slk])
                P.tt("vector", hh[:, fc, :], sl[:], p3_[:, 0:CAP], ALU.mult, reads=[slk, p3k], writes=[(hhk, fc)])
            hks = [(hhk, fc) for fc in range(4)]
            for st_ in range(NS):
                yr, yrk = yrr.next()
                for cg in range(4):
                    py, pyk = psy.next()
                    for fc in range(4):
                        P.mm(py[:, :], hh[:, fc, st_ * 128:(st_ + 1) * 128], w2b[:, fc, cg * 512:(cg + 1) * 512], start=(fc == 0), stop=(fc == 3),
                             reads=hks + w2ks, writes=[pyk])
                    P.copy("scalar" if cg % 2 == 0 else "vector", yr[:, cg * 512:(cg + 1) * 512], py[:, :], reads=[pyk], writes=[(yrk, cg)])
                r0 = e_ * CAP + st_ * 128
                P.dma("sync", Yg[r0:r0 + 128, :], yr[:], reads=[(yrk, cg) for cg in range(4)], writes=[("yg", e_, st_)])
                ykeys.append(("yg", e_, st_))

        cur = load_expert(0)
        for e_ in range(32):
            nxt = load_expert(e_ + 1) if e_ + 1 < 32 else None
            compute_expert(e_, cur)
            cur = nxt

    if debug == 2:
        return dbg_exit()
    with P.scope():
        ln = LN(P, C, D, g, b, "ln")
        y1r = Rot(P, "y1", [128, D], F32, 2)
        zbr = Rot(P, "zb", [128, D], BF16, 2)
        y2r = Rot(P, "y2", [128, D], F32, 2)
        xrr = Rot(P, "xrc", [128, D], F32, 2)
        for ti in range(NT):
            r0 = ti * 128
            y1, y1k = y1r.next()
            y2, y2k = y2r.next()
            for k, (yt, ytk) in enumerate(((y1, y1k), (y2, y2k))):
                P.op("gpsimd", lambda e, yt=yt, ti=ti, k=k: e.indirect_dma_start(
                    out=yt[:], out_offset=None, in_=Yg, in_offset=bass.IndirectOffsetOnAxis(ap=dest_i[:, ti, k:k + 1], axis=0), bounds_check=_bcreg(e), oob_is_err=False),
                    reads=ykeys + [("dest", ti)], writes=[ytk], dma=True)
            xt, xk = xrr.next()
            P.dma("sync", xt[:], xres[r0:r0 + 128, :], writes=[xk])
            P.act(y1[:], y1[:], AF.Identity, scale=gate[:, ti, 0:1], reads=[y1k, ("gate", ti, 0)], writes=[y1k])
            P.stt("vector", y1[:], y2[:], gate[:, ti, 1:2], y1[:], ALU.mult, ALU.add, reads=[y1k, y2k, ("gate", ti, 1)], writes=[y1k])
            P.stt("vector", y1[:], xt[:], ALPHA, y1[:], ALU.mult, ALU.add, reads=[y1k, xk], writes=[y1k])
            _ln_emit_multi(P, ln, y1, [y1k])
            P.dma("sync", y[r0:r0 + 128, :], y1[:], reads=[y1k], writes=[("out", ti)])
            zb, zbk = zbr.next()
            P.copy("scalar", zb[:], y1[:], reads=[y1k], writes=[zbk])
            P.dma("sync", yb[r0:r0 + 128, :], zb[:], reads=[zbk], writes=[("outb", ti)])
    P.build()
    return nc


LAM_INIT0 = 0.8 - 0.6 * 1.0


def build_attn(S):
    NCH = S // 512
    nc = new_nc()
    xT = din(nc, "xT", [D, S])
    wq = din(nc, "wq", [D, 256])
    wk = din(nc, "wk", [D, 256])
    wv = din(nc, "wv", [D, 256])
    wqp = din(nc, "wqp", [D, 64])
    wkp = din(nc, "wkp", [D, 64])
    pos = din(nc, "pos", [1, S], I32)
    rc = din(nc, "rc", [32, 5])
    lamv = din(nc, "lamv", [1, 512])
    subg = din(nc, "subg", [128, 2])
    oT = dout(nc, "oT", [256, S], BF16)
    P = Prog(nc)
    C = setup_consts(P)
    V_, G_ = "vector", "gpsimd"
    trf = P.sb("trf", [128, 128], F32)
    P.memset(G_, trf[:], 1.0, writes=["trf"])
    P.op(G_, lambda e: e.affine_select(trf[:], trf[:], [[1, 128]], ALU.is_ge, 0.0, base=0, channel_multiplier=-1), reads=["trf"], writes=["trf"])
    tri = P.sb("tri", [128, 128], BF16)
    P.copy(V_, tri[:], trf[:], reads=["trf"], writes=["tri"])
    rct = P.sb("rct", [32, 5], F32)
    P.dma("sync", rct[:], rc, writes=["rct"])
    sgt = P.sb("sgt", [128, 2], F32)
    P.dma("sync", sgt[:], subg, writes=["sgt"])
    P.ts(V_, sgt[:], sgt[:], float(1.0 - LAM_INIT0), None, ALU.mult, reads=["sgt"], writes=["sgt"])
    lv = P.sb("lv", [1, 512], F32)
    P.dma("sync", lv[:], lamv, writes=["lv"])
    lt = P.sb("lt", [1, 8], F32)
    P.tt(V_, lv[:, 0:128], lv[:, 0:128], lv[:, 128:256], ALU.mult, reads=["lv"], writes=["lv"])
    P.tt(V_, lv[:, 256:384], lv[:, 256:384], lv[:, 384:512], ALU.mult, reads=["lv"], writes=["lv"])
    P.red(V_, lt[:, 0:1], lv[:, 0:128], ALU.add, reads=["lv"], writes=["lt0"])
    P.red(V_, lt[:, 1:2], lv[:, 256:384], ALU.add, reads=["lv"], writes=["lt1"])
    P.act(lt[:, 2:4], lt[:, 0:2], AF.Exp, reads=["lt0", "lt1"], writes=["lt2"])
    P.tt(V_, lt[:, 4:5], lt[:, 3:4], lt[:, 2:3], ALU.subtract, reads=["lt2"], writes=["lt4"])
    P.ts(V_, lt[:, 5:6], lt[:, 4:5], float(-LAM_INIT0), None, ALU.add, reads=["lt4"], writes=["lt5"])
    sbk = Rot(P, "psS", [128, 512], F32, 5, psum=True)
    obk = Rot(P, "psO", [128, 512], F32, 2, psum=True)
    lbk = Rot(P, "psL", [128, 512], F32, 1, psum=True)
    nlam = P.sb("nlam", [128, 1], F32)
    ps, pk = sbk.next()
    P.mm(ps[:, 0:1], C["ones_f"][0:1, :], lt[0:1, 5:6], reads=["ones_f", "lt5"], writes=[pk])
    P.copy(V_, nlam[:], ps[:, 0:1], reads=[pk], writes=["nlam"])
    wq_bf, wqk = load_w_bf_sw(P, "wq_bf", wq, D, 256)
    wk_bf, wkk = load_w_bf_sw(P, "wk_bf", wk, D, 256)
    wv_bf, wvk = load_w_bf_sw(P, "wv_bf", wv, D, 256)
    wqp_bf, wqpk = load_w_bf_sw(P, "wqp_bf", wqp, D, 64)
    wkp_bf, wkpk = load_w_bf_sw(P, "wkp_bf", wkp, D, 64)
    Kc = P.sb("Kc", [128, 2, S], BF16)
    Vc = P.sb("Vc", [128, S // 128, 256], BF16)
    xTc = P.sb("xTc", [128, 16, 256], BF16)
    qT = P.sb("qT", [128, 2, 512], BF16)
    posi = P.sb("posi", [32, 512], I32)
    posf = P.sb("posf", [32, 512], F32)
    frs = P.sb("frs", [32, 512], F32)
    frsi = posi
    frsf = P.sb("frsf", [32, 512], F32)
    cosT = P.sb("cosT", [32, 512], F32)
    sinT = P.sb("sinT", [32, 512], F32)
    rt1 = Rot(P, "rt1", [32, 256], F32, 1)
    rt2 = Rot(P, "rt2", [32, 256], F32, 1)
    ptr = Rot(P, "pT", [128, 512], BF16, 6)
    rL = P.sb("rL", [128, 512], F32)
    om = [P.sb("om0", [128, 2, 512], F32), P.sb("om1", [128, 2, 512], F32)]
    rstd = P.sb("rstd", [128, 512], F32)
    obf = Rot(P, "obf", [128, 2, 512], BF16, 1)
    xv = xT.rearrange("(kc p) t -> p kc t", p=128)
    oTv = oT.rearrange("(c p) t -> p c t", p=128)
    scale = float(128 ** -0.5)
    for i in range(NCH):
        c0 = i * 512
        P.dma("sync", posi[:], pos[0:1, c0:c0 + 512].partition_broadcast(32), writes=["posi"])
        P.copy(V_, posf[:], posi[:], reads=["posi"], writes=["posf"])
        for which_tab, (tab, shift, scol) in enumerate(((sinT, 0.0, 1), (cosT, 0.25, 2))):
            P.ts(V_, frs[:], posf[:], rct[:, 0:1], shift, ALU.mult, ALU.add, reads=["posf", "rct"], writes=["frs"])
            P.copy(V_, frsi[:], frs[:], reads=["frs", "posf"], writes=["posi"])
            P.copy(V_, frsf[:], frsi[:], reads=["posi"], writes=["frsf"])
            P.tt(V_, frs[:], frs[:], frsf[:], ALU.subtract, reads=["frs", "frsf"], writes=["frs"])
            P.ts(V_, frsf[:], frs[:], 0.5, None, ALU.is_gt, reads=["frs"], writes=["frsf"])
            P.tt(V_, frs[:], frs[:], frsf[:], ALU.subtract, reads=["frs", "frsf"], writes=["frs"])
            P.act(tab[:], frs[:], AF.Sin, scale=rct[:, scol:scol + 1], reads=["frs", "rct"], writes=["sinT" if which_tab == 0 else "cosT"])
        for hf in range(2):
            h0 = hf * 256
            for q4 in range(4):
                P.dma("gpsimd", xTc[:, q4 * 4:(q4 + 1) * 4, :], xv[:, q4 * 4:(q4 + 1) * 4, c0 + h0:c0 + h0 + 256], writes=[("xTc", q4)])
            xks = [("xTc", q4) for q4 in range(4)]
            for which in range(2):
                w_bf, wks, wp_bf, wpks = (wq_bf, wqk, wqp_bf, wqpk) if which == 0 else (wk_bf, wkk, wkp_bf, wkpk)
                for m in range(2):
                    dst = qT[:, m, h0:h0 + 256] if which == 0 else Kc[:, m, c0 + h0:c0 + h0 + 256]
                    dk = ("qT", m, hf) if which == 0 else ("Kc", m, i, hf)
                    pm, pmk = sbk.next()
                    for kc in range(16):
                        P.mm(pm[:, 0:256], w_bf[:, kc, m * 128:(m + 1) * 128], xTc[:, kc, :], start=(kc == 0), stop=(kc == 15), reads=wks + xks, writes=[pmk])
                    pp, ppk = sbk.next()
                    for kc in range(16):
                        P.mm(pp[0:32, 0:256], wp_bf[:, kc, m * 32:(m + 1) * 32], xTc[:, kc, :], start=(kc == 0), stop=(kc == 15), reads=wpks + xks, writes=[ppk])
                    P.copy("scalar", dst[32:64, :], pm[32:64, 0:256], reads=[pmk], writes=[(dk, "hi0")])
                    P.copy("scalar", dst[64:128, :], pm[64:128, 0:256], reads=[pmk], writes=[(dk, "hi")])
                    t1, t1k = rt1.next()
                    t2, t2k = rt2.next()
                    P.tt(V_, t1[:], pm[0:32, 0:256], cosT[:, h0:h0 + 256], ALU.mult, reads=[pmk, "cosT"], writes=[t1k])
                    P.tt(V_, t2[:], pp[0:32, 0:256], sinT[:, h0:h0 + 256], ALU.mult, reads=[ppk, "sinT"], writes=[t2k])
                    P.tt(G_, dst[0:32, :], t1[:], t2[:], ALU.add, reads=[t1k, t2k], writes=[(dk, "lo")])
            for t2_ in range(2):
                tt = hf * 2 + t2_
                pv, pvk = sbk.next()
                for kc in range(16):
                    P.mm(pv[:, 0:256], xTc[:, kc, t2_ * 128:(t2_ + 1) * 128], wv_bf[:, kc, :], start=(kc == 0), stop=(kc == 15), reads=wvk + xks, writes=[pvk])
                P.copy("scalar" if tt % 2 == 0 else V_, Vc[:, i * 4 + tt, :], pv[:, 0:256], reads=[pvk], writes=[("Vc", i * 4 + tt)])
        jlist = [(4 * i + o, o) for o in range(4)] + [(j, -1) for j in range(4 * i)]
        LA = 3
        for m in range(2):
            Ob = [obk.next() for dvc in range(2)]
            lb, lbk_ = lbk.next()
            pend = []

            def emit_qk(j, o, m=m):
                n0 = o * 128 if o > 0 else 0
                jc = j // 4
                ps, pk = sbk.next()
                P.mm(ps[:, n0:512], Kc[:, m, j * 128:(j + 1) * 128], qT[:, m, n0:512],
                     reads=[(("Kc", m, jc, hf_), part) for hf_ in range(2) for part in ("hi", "hi0", "lo")]
                     + [(("qT", m, hf_), part) for hf_ in range(2) for part in ("hi", "hi0", "lo")], writes=[pk])
                pt, ptk = ptr.next()
                P.act(pt[:, n0:512], ps[:, n0:512], AF.Exp, scale=scale, reads=[pk], writes=[ptk])
                if o >= 0:
                    P.tt(G_, pt[:, n0:n0 + 128], pt[:, n0:n0 + 128], tri[:], ALU.mult, reads=[ptk, "tri"], writes=[ptk])
                return (j, n0, pt, ptk)

            def emit_pv(item, first, last, Ob=Ob, lb=lb, lbk_=lbk_):
                j, n0, pt, ptk = item
                for dvc in range(2):
                    ob, obk_ = Ob[dvc]
                    P.mm(ob[:, n0:512], Vc[:, j, dvc * 128:(dvc + 1) * 128], pt[:, n0:512], start=first, stop=last,
                         reads=[("Vc", j), ptk], writes=[obk_], skip_group_check=True)
                P.mm(lb[:, n0:512], C["ones_b"][:], pt[:, n0:512], start=first, stop=last, reads=["ones_b", ptk], writes=[lbk_], skip_group_check=True)

            ndone = 0
            for (j, o) in jlist:
                pend.append(emit_qk(j, o))
                if len(pend) > LA:
                    emit_pv(pend.pop(0), ndone == 0, False)
                    ndone += 1
            while pend:
                emit_pv(pend.pop(0), ndone == 0, len(pend) == 0)
                ndone += 1
            P.recip(rL[:], lb[:, :], reads=[lbk_], writes=["rL"])
            for dvc in range(2):
                P.tt(V_, om[m][:, dvc, :], Ob[dvc][0][:, :], rL[:], ALU.mult, reads=[Ob[dvc][1], "rL"], writes=[("om", m, dvc)])
        for dvc in range(2):
            P.stt(V_, om[1][:, dvc, :], om[1][:, dvc, :], nlam[:, 0:1], om[0][:, dvc, :], ALU.mult, ALU.add,
                  reads=[("om", 0, dvc), ("om", 1, dvc), "nlam"], writes=[("om", 1, dvc)])
        for dvc in range(2):
            P.act(om[0][:, dvc, :], om[1][:, dvc, :], AF.Square, reads=[("om", 1, dvc)], writes=[("om", 0, dvc)])
        ps, pk = sbk.next()
        for dvc in range(2):
            P.mm(ps[:, :], C["ones_f"][:], om[0][:, dvc, :], start=(dvc == 0), stop=(dvc == 1), reads=["ones_f", ("om", 0, dvc)], writes=[pk])
        P.act(rstd[:], ps[:, :], AF.Sqrt, scale=float(1.0 / 256.0), bias=C["eps"][:], reads=[pk, "eps"], writes=["rstd"])
        P.recip(rstd[:], rstd[:], reads=["rstd"], writes=["rstd"])
        ob_, obfk = obf.next()
        for dvc in range(2):
            P.tt(G_, om[1][:, dvc, :], om[1][:, dvc, :], rstd[:], ALU.mult, reads=[("om", 1, dvc), "rstd"], writes=[("om", 1, dvc)])
            P.act(ob_[:, dvc, :], om[1][:, dvc, :], AF.Identity, scale=sgt[:, dvc:dvc + 1], reads=[("om", 1, dvc), "sgt"], writes=[(obfk, dvc)])
        P.dma("sync", oTv[:, :, c0:c0 + 512], ob_[:], reads=[(obfk, 0), (obfk, 1)], writes=[("oT", i)])
    P.build()
    return nc


def build_conf(T):
    HAL = 128
    nc = new_nc()
    xTh = din(nc, "xTh", [D, HAL + T])
    wglu = din(nc, "wglu", [D, 2048])
    convw = din(nc, "convw", [128, 8 * 31])
    cvec = din(nc, "cvec", [128, 24])
    cm = dout(nc, "cmT", [1024, T], BF16)
    P = Prog(nc)
    C = setup_consts(P)
    V_, G_ = "vector", "gpsimd"
    cwt = P.sb("cwt", [128, 8, 31], F32)
    P.dma("sync", cwt[:], convw.rearrange("p (c j) -> p c j", j=31), writes=["cwt"])
    cvt = P.sb("cvt", [128, 24], F32)
    P.dma("sync", cvt[:], cvec, writes=["cvt"])
    cT = P.sb("cT", [128, 8, HAL + T], BF16)
    xv = xTh.rearrange("(kc p) t -> p kc t", p=128)
    chunks = [(0, HAL)] + [(HAL + i * 512, 512) for i in range(T // 512)]
    with P.scope():
        w_bf, wks = load_w_bf(P, "wglu_bf", wglu, D, 2048)
        xcr = Rot(P, "xTc", [128, 16, 512], BF16, 2)
        sgr = Rot(P, "sig", [128, 512], F32, 2)
        pa_r = Rot(P, "psa", [128, 512], F32, 4, psum=True)
        pg_r = Rot(P, "psg", [128, 512], F32, 4, psum=True)
        for ci, (c0, n) in enumerate(chunks):
            xc, xk = xcr.next()
            for q4 in range(4):
                P.dma("gpsimd", xc[:, q4 * 4:(q4 + 1) * 4, 0:n], xv[:, q4 * 4:(q4 + 1) * 4, c0:c0 + n], writes=[(xk, q4)])
            xks = [(xk, q4) for q4 in range(4)]
            for cc in range(8):
                pa, pak = pa_r.next()
                for kc in range(16):
                    P.mm(pa[:, 0:n], w_bf[:, kc, cc * 128:(cc + 1) * 128], xc[:, kc, 0:n], start=(kc == 0), stop=(kc == 15), reads=wks + xks, writes=[pak])
                pg, pgk = pg_r.next()
                for kc in range(16):
                    P.mm(pg[:, 0:n], w_bf[:, kc, 1024 + cc * 128:1024 + (cc + 1) * 128], xc[:, kc, 0:n], start=(kc == 0), stop=(kc == 15), reads=wks + xks, writes=[pgk])
                sg, sgk = sgr.next()
                P.act(sg[:, 0:n], pg[:, 0:n], AF.Sigmoid, reads=[pgk], writes=[sgk])
                P.tt(V_, cT[:, cc, c0:c0 + n], pa[:, 0:n], sg[:, 0:n], ALU.mult, reads=[pak, sgk], writes=[("cT", cc, ci)])
    with P.scope():
        dg = P.sb("diag", [128, 8 * 31, 128], BF16)
        for cc in range(8):
            for j in range(31):
                P.ts(V_ if (j % 2 == 0) else G_, dg[:, cc * 31 + j, :], C["ident_b"][:], cwt[:, cc, j:j + 1], None, ALU.mult,
                     reads=["ident_b", "cwt"], writes=[("dg", cc, j)])
        convs = P.sb("convs", [128, 8, 512], F32)
        sqr = Rot(P, "sq", [128, 512], F32, 2)
        mean = P.sb("mean", [128, 512], F32)
        msq = P.sb("msq", [128, 512], F32)
        rstd = P.sb("rstd", [128, 512], F32)
        tmr = Rot(P, "tm", [128, 512], F32, 2)
        obr = Rot(P, "ob", [128, 8, 512], BF16, 2)
        pcr = Rot(P, "psc", [128, 512], F32, 4, psum=True)
        ps1r = Rot(P, "ps1", [128, 512], F32, 2, psum=True)
        ps2r = Rot(P, "ps2", [128, 512], F32, 2, psum=True)
        cmv = cm.rearrange("(c p) t -> p c t", p=128)
        for tc in range(T // 512):
            t0 = HAL + tc * 512
            cidx = [ci for ci, (c0, n) in enumerate(chunks) if c0 < t0 + 512 and c0 + n > t0 - 30]
            s1, s1k = ps1r.next()
            s2, s2k = ps2r.next()
            for cc in range(8):
                pc, pck = pcr.next()
                for j in range(31):
                    P.mm(pc[:, :], dg[:, cc * 31 + j, :], cT[:, cc, t0 - 30 + j:t0 - 30 + j + 512], start=(j == 0), stop=(j == 30),
                         reads=[("dg", cc, j)] + [("cT", cc, ci) for ci in cidx], writes=[pck])
                P.act(convs[:, cc, :], pc[:, :], AF.Identity, bias=cvt[:, cc:cc + 1], scale=1.0, reads=[pck, "cvt"], writes=[("convs", cc)])
                sq, sqk = sqr.next()
                P.tt(G_, sq[:], convs[:, cc, :], convs[:, cc, :], ALU.mult, reads=[("convs", cc)], writes=[sqk])
                P.mm(s1[:, :], C["ones_f"][:], convs[:, cc, :], start=(cc == 0), stop=(cc == 7), reads=["ones_f", ("convs", cc)], writes=[s1k])
                P.mm(s2[:, :], C["ones_f"][:], sq[:], start=(cc == 0), stop=(cc == 7), reads=["ones_f", sqk], writes=[s2k])
            P.ts(V_, mean[:], s1[:, :], float(1.0 / 1024), None, ALU.mult, reads=[s1k], writes=["mean"])
            P.tt(V_, msq[:], mean[:], mean[:], ALU.mult, reads=["mean"], writes=["msq"])
            P.stt(V_, msq[:], s2[:, :], float(1.0 / 1024), msq[:], ALU.mult, ALU.subtract, reads=[s2k, "msq"], writes=["msq"])
            P.act(rstd[:], msq[:], AF.Sqrt, bias=C["eps"][:], scale=1.0, reads=["msq", "eps"], writes=["rstd"])
            P.recip(rstd[:], rstd[:], reads=["rstd"], writes=["rstd"])
            ob, obk = obr.next()
            for cc in range(8):
                tm, tmk = tmr.next()
                P.tt(G_, tm[:], convs[:, cc, :], mean[:], ALU.subtract, reads=[("convs", cc), "mean"], writes=[tmk])
                P.tt(V_, tm[:], tm[:], rstd[:], ALU.mult, reads=[tmk, "rstd"], writes=[tmk])
                P.act(ob[:, cc, :], tm[:], AF.Silu, scale=cvt[:, 8 + cc:9 + cc], bias=cvt[:, 16 + cc:17 + cc], reads=[tmk, "cvt"], writes=[(obk, cc)])
            P.dma("sync", cmv[:, :, tc * 512:(tc + 1) * 512], ob[:], reads=[(obk, cc) for cc in range(8)], writes=[("cm", tc)])
    P.build()
    return nc


def build_sgu(T):
    nc = new_nc()
    xT = din(nc, "xT", [D, T], BF16)
    wuv = din(nc, "wuv", [D, 2048])
    lg_ = din(nc, "lng", [1, 1024])
    lb_ = din(nc, "lnb", [1, 1024])
    swT = din(nc, "swT", [128, 4 * 128])
    sgb = din(nc, "sgb", [1, 512])
    sp = dout(nc, "spT", [1024, T], BF16)
    P = Prog(nc)
    C = setup_consts(P)
    V_, G_ = "vector", "gpsimd"
    ln = LN(P, C, 1024, lg_, lb_, "sln")
    wf = P.sb("swf", [128, 4, 128], F32)
    P.dma("sync", wf[:], swT.rearrange("p (g t) -> p g t", t=128), writes=["swf"])
    for gi in range(4):
        P.op(G_, lambda e, gi=gi: e.affine_select(wf[:, gi, :], wf[:, gi, :], [[1, 128]], ALU.is_ge, 0.0, base=0, channel_multiplier=-1),
             reads=["swf"], writes=["swf"])
    wc = P.sb("swc", [128, 4, 128], BF16)
    P.copy(V_, wc[:], wf[:], reads=["swf"], writes=["swc"])
    bT = load_bcast(P, "sbT", sgb, 512)
    bT8 = P.sb("bT8", [128, 8, 128], F32)
    for cc in range(8):
        P.copy(V_, bT8[:, cc, :], bT[:, (cc // 2) * 128:(cc // 2 + 1) * 128], reads=["sbT"], writes=[("bT8", cc)])
    b8k = [("bT8", cc) for cc in range(8)]
    w_bf, wks = load_w_bf(P, "wuv_bf", wuv, D, 2048)
    xcr = Rot(P, "xTc", [128, 16, 512], BF16, 2)
    uT = P.sb("uT", [128, 8, 512], F32)
    vzr = Rot(P, "vz", [128, 1024], F32, 2)
    vgr = Rot(P, "vg", [128, 1024], BF16, 2)
    tmr = Rot(P, "stm", [128, 4, 128], F32, 2)
    spr = Rot(P, "spo", [128, 8, 512], BF16, 2)
    pur = Rot(P, "psu", [128, 512], F32, 2, psum=True)
    pvr = Rot(P, "psv", [128, 512], F32, 2, psum=True)
    psr = Rot(P, "pss", [128, 512], F32, 4, psum=True)
    xv = xT.rearrange("(kc p) t -> p kc t", p=128)
    spv = sp.rearrange("(c p) t -> p c t", p=128)
    for tc in range(T // 512):
        xc, xk = xcr.next()
        for q4 in range(4):
            P.dma("sync", xc[:, q4 * 4:(q4 + 1) * 4, :], xv[:, q4 * 4:(q4 + 1) * 4, tc * 512:(tc + 1) * 512], writes=[(xk, q4)])
        xks = [(xk, q4) for q4 in range(4)]
        for cc in range(8):
            pu, puk = pur.next()
            for kc in range(16):
                P.mm(pu[:, :], w_bf[:, kc, cc * 128:(cc + 1) * 128], xc[:, kc, :], start=(kc == 0), stop=(kc == 15), reads=wks + xks, writes=[puk])
            P.act(uT[:, cc, :], pu[:, :], AF.Gelu, reads=[puk], writes=[("uT", cc)])
        so, sok = spr.next()
        for tt in range(4):
            vz, vzk = vzr.next()
            for hf in range(2):
                pv, pvk = pvr.next()
                for kc in range(16):
                    P.mm(pv[:, :], xc[:, kc, tt * 128:(tt + 1) * 128], w_bf[:, kc, 1024 + hf * 512:1024 + (hf + 1) * 512], start=(kc == 0), stop=(kc == 15),
                         reads=wks + xks, writes=[pvk])
                P.act(vz[:, hf * 512:(hf + 1) * 512], pv[:, :], AF.Gelu, reads=[pvk], writes=[(vzk, hf)])
            vzks = [(vzk, 0), (vzk, 1)]
            _ln_emit_multi(P, ln, vz, vzks)
            vg, vgk = vgr.next()
            P.copy("scalar", vg[:], vz[:], reads=vzks, writes=[vgk])
            for hb in range(2):
                pb, pbk = psr.next()
                for c4 in range(4):
                    cc = hb * 4 + c4
                    P.mm(pb[:, c4 * 128:(c4 + 1) * 128], vg[:, cc * 128:(cc + 1) * 128], wc[:, cc // 2, :], reads=[vgk, "swc"], writes=[pbk])
                tm, tmk = tmr.next()
                P.tt(V_, tm[:], pb[:, :].rearrange("p (c t) -> p c t", t=128), bT8[:, hb * 4:(hb + 1) * 4, :], ALU.add, reads=[pbk] + b8k, writes=[tmk])
                P.tt(V_, so[:, hb * 4:(hb + 1) * 4, tt * 128:(tt + 1) * 128], tm[:], uT[:, hb * 4:(hb + 1) * 4, tt * 128:(tt + 1) * 128], ALU.mult,
                     reads=[tmk] + [("uT", hb * 4 + c4) for c4 in range(4)], writes=[(sok, tt, hb)])
        P.dma("sync", spv[:, :, tc * 512:(tc + 1) * 512], so[:], reads=[(sok, tt, hb) for tt in range(4) for hb in range(2)], writes=[("sp", tc)])
    P.build()
    return nc


def build_sconv(T):
    nc = new_nc()
    xTh = din(nc, "xTh", [D, 2 + T], BF16)
    wsc = din(nc, "wsc", [D, 3072])
    scw = din(nc, "scw", [128, 24])
    cv = dout(nc, "cvT", [1024, T], BF16)
    P = Prog(nc)
    C = setup_consts(P)
    V_, G_ = "vector", "gpsimd"
    swt = P.sb("scwt", [128, 8, 3], F32)
    P.dma("sync", swt[:], scw.rearrange("p (c j) -> p c j", j=3), writes=["scwt"])
    w_bf, wks = load_w_bf(P, "wsc_bf", wsc, D, 3072)
    xcr = Rot(P, "xTc", [128, 16, 512], BF16, 2)
    prod = P.sb("prod", [128, 8, 514], F32)
    gcr = Rot(P, "gcs", [128, 512], F32, 2)
    acr = Rot(P, "acc", [128, 512], F32, 2)
    obr = Rot(P, "ob", [128, 8, 512], BF16, 2)
    pbr = Rot(P, "psb", [128, 512], F32, 2, psum=True)
    pcr = Rot(P, "psc", [128, 512], F32, 3, psum=True)
    pxr = Rot(P, "psx", [128, 512], F32, 3, psum=True)
    xv = xTh.rearrange("(kc p) t -> p kc t", p=128)
    cvv = cv.rearrange("(c p) t -> p c t", p=128)
    chunks = [(0, 2, -1)] + [(2 + i * 512, 512, i) for i in range(T // 512)]
    for (c0, n, tc) in chunks:
        xc, xk = xcr.next()
        for q4 in range(4):
            P.dma("sync", xc[:, q4 * 4:(q4 + 1) * 4, 0:n], xv[:, q4 * 4:(q4 + 1) * 4, c0:c0 + n], writes=[(xk, q4)])
        xks = [(xk, q4) for q4 in range(4)]
        if tc >= 0:
            ob, obk = obr.next()
        for cc in range(8):
            pc, pck = pcr.next()
            for kc in range(16):
                P.mm(pc[:, 0:n], w_bf[:, kc, 1024 + cc * 128:1024 + (cc + 1) * 128], xc[:, kc, 0:n], start=(kc == 0), stop=(kc == 15), reads=wks + xks, writes=[pck])
            px, pxk = pxr.next()
            for kc in range(16):
                P.mm(px[:, 0:n], w_bf[:, kc, 2048 + cc * 128:2048 + (cc + 1) * 128], xc[:, kc, 0:n], start=(kc == 0), stop=(kc == 15), reads=wks + xks, writes=[pxk])
            gs, gsk = gcr.next()
            P.copy("scalar", gs[:, 0:n], pc[:, 0:n], reads=[pck], writes=[gsk])
            pk_ = ("prod", cc)
            if tc < 0:
                P.tt(V_, prod[:, cc, 0:2], gs[:, 0:2], px[:, 0:2], ALU.mult, reads=[gsk, pxk], writes=[pk_])
                continue
            pb, pbk = pbr.next()
            for kc in range(16):
                P.mm(pb[:, :], w_bf[:, kc, cc * 128:(cc + 1) * 128], xc[:, kc, :], start=(kc == 0), stop=(kc == 15), reads=wks + xks, writes=[pbk])
            P.tt(V_, prod[:, cc, 2:514], gs[:], px[:, :], ALU.mult, reads=[gsk, pxk, pk_], writes=[pk_])
            ac, ack = acr.next()
            P.act(ac[:], prod[:, cc, 2:514], AF.Identity, scale=swt[:, cc, 2:3], reads=[pk_, "scwt"], writes=[ack])
            P.stt(V_, ac[:], prod[:, cc, 1:513], swt[:, cc, 1:2], ac[:], ALU.mult, ALU.add, reads=[pk_, "scwt", ack], writes=[ack])
            P.stt(V_, ac[:], prod[:, cc, 0:512], swt[:, cc, 0:1], ac[:], ALU.mult, ALU.add, reads=[pk_, "scwt", ack], writes=[ack])
            P.tt(V_, ob[:, cc, :], ac[:], pb[:, :], ALU.mult, reads=[ack, pbk], writes=[(obk, cc)])
            P.copy(G_, prod[:, cc, 0:2], prod[:, cc, 512:514], reads=[pk_], writes=[pk_])
        if tc >= 0:
            P.dma("sync", cvv[:, :, tc * 512:(tc + 1) * 512], ob[:], reads=[(obk, cc) for cc in range(8)], writes=[("cv", tc)])
    P.build()
    return nc


B_, S_, T_ = 2, 16384, 4096
NCORE = 8


def _rope_consts():
    inv = (500000.0 ** (-np.arange(0, 32, 2, dtype=np.float32) / 32)).astype(np.float32)
    PI = 3.1415925
    rc = np.zeros((32, 5), np.float32)
    for p in range(32):
        s = -1.0 if p < 16 else 1.0
        rc[p] = [inv[p % 16] / (2 * np.pi), s * 2 * PI, 2 * PI, 0, 0]
    return rc


def _perm_cols(w):
    return np.ascontiguousarray(np.concatenate(
        [np.concatenate([w[:, m * 128 + 16:m * 128 + 32], w[:, m * 128:m * 128 + 16]], 1) for m in range(2)], 1))


def _pl(v, n):
    return np.ascontiguousarray(np.asarray(v).reshape(n, 128).T)


def _run(nc, in_maps):
    res = run_bass_kernel_spmd(nc, in_maps, core_ids=list(range(NCORE)))
    return res.results


def _c(a):
    return np.ascontiguousarray(a)


def kernel(**inp):
    f32 = np.float32
    x = np.asarray(inp["x"], f32)
    mem = np.asarray(inp["mem"], f32)
    positions = np.asarray(inp["positions"], np.int32)
    w_in = np.asarray(inp["w_in"], f32)
    w_out = np.asarray(inp["w_out"], f32)
    cores = [(c // 4, c % 4) for c in range(NCORE)]

    xTb = [_c(x[b].T) for b in range(B_)]
    w0 = w_in[0]
    lamv = _c(np.concatenate([inp["lam_q1"][0], inp["lam_k1"][0], inp["lam_q2"][0], inp["lam_k2"][0]])[None].astype(f32))
    subg = _pl(inp["diff_subln_g"][0].astype(f32), 2)
    rc = _rope_consts()
    maps = []
    for (b, h) in cores:
        wq = _c(w0[:, h * 256:(h + 1) * 256])
        wk = _c(w0[:, 1024 + h * 256:1024 + (h + 1) * 256])
        wv = _c(w0[:, 2048 + h * 256:2048 + (h + 1) * 256])
        maps.append(dict(xT=xTb[b], wq=wq, wk=wk, wv=wv, wqp=_perm_cols(wq), wkp=_perm_cols(wk), pos=_c(positions[b][None]),
                         rc=rc, lamv=lamv, subg=subg))
    r = _run(build_attn(S_), maps)
    oT = [[r[b * 4 + h]["oT"] for h in range(4)] for b in range(B_)]

    cw = np.asarray(inp["conv_w"][0], f32)
    convw = _c(cw.T.reshape(8, 128, 31).transpose(1, 0, 2).reshape(128, 8 * 31))
    cvec = _c(np.concatenate([_pl(inp["conv_b"][0].astype(f32), 8), _pl(inp["conv_ln_g"][0].astype(f32), 8), _pl(inp["conv_ln_b"][0].astype(f32), 8)], 1))
    wglu = _c(w0[:, 3072:5120])
    maps = []
    for (b, rr) in cores:
        xh = np.zeros((D, 128 + T_), f32)
        lo = rr * T_ - 128
        if lo >= 0:
            xh[:] = xTb[b][:, lo:(rr + 1) * T_]
        else:
            xh[:, 128:] = xTb[b][:, 0:T_]
        maps.append(dict(xTh=xh, wglu=wglu, convw=convw, cvec=cvec))
    r = _run(build_conf(T_), maps)
    cmT = [r[c]["cmT"] for c in range(NCORE)]

    nc_out = build_outproj(T_)
    nc_cross = build_cross(T_)
    nc_moe = build_moe(T_)
    memT = [_c(mem[b].T) for b in range(B_)]
    kvw = np.asarray(inp["mem_kv_w"], f32)
    ecap = (np.arange(32, dtype=f32) * CAP)[None]

    def tail(l, mixT, xcur):
        maps = [dict(mixT=mixT[c], xres=xcur[c], w=_c(w_out[l]), g=_c(inp["ln_mix_g"][l][None].astype(f32)), b=_c(inp["ln_mix_b"][l][None].astype(f32)))
                for c in range(NCORE)]
        r = _run(nc_out, maps)
        x1 = [r[c]["y"] for c in range(NCORE)]
        x1b = [r[c]["yb"] for c in range(NCORE)]
        maps = [dict(xT=_c(x1b[c].T), xres=x1[c], memT=memT[cores[c][0]], kvw=kvw, xqw=_c(inp["xq_w"][l].astype(f32)), xow=_c(inp["xo_w"][l].astype(f32)),
                     g=_c(inp["ln_mem_g"][l][None].astype(f32)), b=_c(inp["ln_mem_b"][l][None].astype(f32))) for c in range(NCORE)]
        r = _run(nc_cross, maps)
        x2 = [r[c]["y"] for c in range(NCORE)]
        rw = _c(np.concatenate([inp["rg_w"][l], inp["re_w"][l]], 1).astype(f32))
        rb = _c(np.concatenate([inp["rg_b"][l], inp["re_b"][l]])[None].astype(f32))
        w1, w3, w2 = _c(inp["e_w1"][l].astype(f32)), _c(inp["e_w3"][l].astype(f32)), _c(inp["e_w2"][l].astype(f32))
        maps = [dict(xT=_c(x2[c].T), xres=x2[c], rw=rw, rb=rb, ecap=ecap, w1=w1, w3=w3, w2=w2,
                     g=_c(inp["ln_ffn_g"][l][None].astype(f32)), b=_c(inp["ln_ffn_b"][l][None].astype(f32))) for c in range(NCORE)]
        r = _run(nc_moe, maps)
        print("[moe] layer", l, "max per-core per-expert load:", max(float(r[c]["cnt"].max()) for c in range(NCORE)), "capacity", CAP, flush=True)
        return [r[c]["y"] for c in range(NCORE)], [r[c]["yb"] for c in range(NCORE)]

    mix0 = [_c(np.concatenate([oT[b][h][:, rr * T_:(rr + 1) * T_] for h in range(4)] + [cmT[c]], 0)) for c, (b, rr) in enumerate(cores)]
    xcur = [_c(x[b, rr * T_:(rr + 1) * T_]) for (b, rr) in cores]
    xl0, xl0b = tail(0, mix0, xcur)

    w1_ = w_in[1]
    sw = np.asarray(inp["sgu_w"][0], f32)
    swT = _c(sw.transpose(2, 0, 1).reshape(128, 512))
    sgb = _c(np.asarray(inp["sgu_b"][0], f32).reshape(1, 512))
    xl0T = [_c(a.T) for a in xl0b]
    maps = [dict(xT=xl0T[c], wuv=_c(w1_[:, 0:2048]), lng=_c(inp["sgu_ln_g"][0][None].astype(f32)), lnb=_c(inp["sgu_ln_b"][0][None].astype(f32)),
                 swT=swT, sgb=sgb) for c in range(NCORE)]
    r = _run(build_sgu(T_), maps)
    spT = [r[c]["spT"] for c in range(NCORE)]
    scw = _c(np.asarray(inp["sc_w"][0], f32).T.reshape(8, 128, 3).transpose(1, 0, 2).reshape(128, 24))
    maps = []
    for c, (b, rr) in enumerate(cores):
        xh = np.zeros((D, 2 + T_), xl0T[c].dtype)
        xh[:, 2:] = xl0T[c]
        if rr > 0:
            xh[:, 0:2] = xl0T[c - 1][:, T_ - 2:T_]
        maps.append(dict(xTh=xh, wsc=_c(w1_[:, 2048:5120]), scw=scw))
    r = _run(build_sconv(T_), maps)
    cvT = [r[c]["cvT"] for c in range(NCORE)]
    mix1 = [_c(np.concatenate([spT[c], cvT[c]], 0)) for c in range(NCORE)]
    xl1, _unused = tail(1, mix1, xl0)
    out = np.zeros((B_, S_, D), f32)
    for c, (b, rr) in enumerate(cores):
        out[b, rr * T_:(rr + 1) * T_] = xl1[c]
    return out
```

```python
import numpy as np
import concourse.bass as bass
import concourse.mybir as mybir
from concourse.bass_utils import run_bass_kernel_spmd
from contextlib import ExitStack, contextmanager

F32 = mybir.dt.float32
BF16 = mybir.dt.bfloat16
I32 = mybir.dt.int32
ALU = mybir.AluOpType
AF = mybir.ActivationFunctionType
AX = mybir.AxisListType

COMPUTE = ("tensor", "vector", "scalar", "gpsimd")
ENGS = ("tensor", "vector", "scalar", "gpsimd", "sync")
NDSEM = 6
SB_LIMIT = 228000


class Op:
    __slots__ = ("eng", "fn", "reads", "writes", "dma", "deps", "signal", "seq", "didx", "waits")

    def __init__(self, eng, fn, reads, writes, dma):
        self.eng = eng
        self.fn = fn
        self.reads = tuple(reads)
        self.writes = tuple(writes)
        self.dma = dma
        self.deps = set()
        self.signal = False
        self.seq = 0
        self.didx = -1
        self.waits = []


class Prog:
    def __init__(self, nc):
        self.nc = nc
        self.ops = []
        self.stack = ExitStack()
        self.npsum = 0
        self.sb_off = 16384
        self.sb_peak = 0
        self.nalloc = 0

    def sb(self, name, shape, dt):
        esz = {F32: 4, BF16: 2, I32: 4}[dt]
        n = 1
        for d in shape[1:]:
            n *= d
        nbytes = (n * esz + 63) // 64 * 64
        off = self.sb_off
        assert off + nbytes <= SB_LIMIT, ("SBUF overflow", name, off, nbytes)
        self.sb_off = off + nbytes
        self.sb_peak = max(self.sb_peak, self.sb_off)
        self.nalloc += 1
        return self.nc.alloc_sbuf_tensor_at("%s_%d" % (name, self.nalloc), list(shape), dt, offset=off)

    @contextmanager
    def scope(self):
        self.barrier()
        save = self.sb_off
        try:
            yield
        finally:
            self.sb_off = save
            self.barrier()

    def ps(self, name, shape, dt=F32):
        return self.stack.enter_context(self.nc.psum_tensor(name, list(shape), dt))

    def op(self, eng, fn, reads=(), writes=(), dma=False):
        o = Op(eng, fn, reads, writes, dma)
        self.ops.append(o)
        return o

    def dma(self, eng, out, in_, reads=(), writes=(), **kw):
        return self.op(eng, lambda e: e.dma_start(out=out, in_=in_, **kw), reads, writes, dma=True)

    def mm(self, out, lhsT, rhs, start=True, stop=True, reads=(), writes=(), **kw):
        return self.op("tensor", lambda e: e.matmul(out, lhsT, rhs, start=start, stop=stop, **kw), reads, writes)

    def tr(self, out, in_, ident, reads=(), writes=()):
        return self.op("tensor", lambda e: e.transpose(out, in_, ident), reads, writes)

    def act(self, out, in_, func, reads=(), writes=(), **kw):
        return self.op("scalar", lambda e: e.activation(out, in_, func, **kw), reads, writes)

    def barrier(self):
        self.ops.append(Op(None, None, (), (), False))

    def tt(self, eng, out, in0, in1, op, reads=(), writes=()):
        return self.op(eng, lambda e: e.tensor_tensor(out, in0, in1, op), reads, writes)

    def ts(self, eng, out, in0, s1, s2, op0, op1=None, reads=(), writes=(), **kw):
        if op1 is None:
            return self.op(eng, lambda e: e.tensor_scalar(out, in0, s1, s2, op0, **kw), reads, writes)
        return self.op(eng, lambda e: e.tensor_scalar(out, in0, s1, s2, op0, op1, **kw), reads, writes)

    def stt(self, eng, out, in0, scalar, in1, op0, op1, reads=(), writes=()):
        return self.op(eng, lambda e: e.scalar_tensor_tensor(out, in0, scalar, in1, op0, op1), reads, writes)

    def copy(self, eng, out, in_, reads=(), writes=()):
        if eng == "scalar":
            return self.op(eng, lambda e: e.copy(out, in_), reads, writes)
        return self.op(eng, lambda e: e.tensor_copy(out, in_), reads, writes)

    def memset(self, eng, ap, val, writes=()):
        return self.op(eng, lambda e: e.memset(ap, val), (), writes)

    def recip(self, out, in_, reads=(), writes=()):
        return self.op("vector", lambda e: e.reciprocal(out, in_), reads, writes)

    def red(self, eng, out, in_, op, reads=(), writes=()):
        return self.op(eng, lambda e: e.tensor_reduce(out, in_, AX.X, op), reads, writes)

    def build(self):
        nc = self.nc
        ops = self.ops
        last_w = {}
        readers = {}
        pend = {}
        lastc = {}
        lastd = {e: [] for e in ENGS}
        for i, o in enumerate(ops):
            if o.eng is None:
                bd = set(lastc.values())
                for e in ENGS:
                    bd.update(lastd[e][-NDSEM:])
                for e in ENGS:
                    pend[e] = set(bd) | pend.get(e, set())
                continue
            deps = set(pend.pop(o.eng, ()))
            if o.dma:
                lastd[o.eng].append(i)
            else:
                lastc[o.eng] = i
            for k in o.reads:
                w = last_w.get(k)
                if w is not None:
                    deps.add(w)
            for k in o.writes:
                w = last_w.get(k)
                if w is not None:
                    deps.add(w)
                deps.update(readers.get(k, ()))
            deps.discard(i)
            for k in o.writes:
                last_w[k] = i
                readers[k] = []
            for k in o.reads:
                readers.setdefault(k, []).append(i)
            o.deps = {d for d in deps if not (o.eng == "tensor" and ops[d].eng == "tensor" and not o.dma and not ops[d].dma)}
            for d in o.deps:
                ops[d].signal = True
        st = self.stack
        csem = {e: st.enter_context(nc.semaphore("c_" + e)) for e in COMPUTE}
        dsem = {e: [st.enter_context(nc.semaphore("d_%s%d" % (e, j))) for j in range(NDSEM)] for e in ENGS}
        cseq = {e: 0 for e in COMPUTE}
        dcnt = {e: 0 for e in ENGS}
        ops_all = ops
        ops = [o for o in ops_all if o.eng is not None]
        for o in ops:
            if o.dma:
                o.didx = dcnt[o.eng]
                dcnt[o.eng] += 1
            elif o.signal:
                cseq[o.eng] += 1
                o.seq = cseq[o.eng]

        def done_cond(d):
            od = ops_all[d]
            if od.dma:
                return (dsem[od.eng][od.didx % NDSEM], 16 * (od.didx // NDSEM + 1))
            return (csem[od.eng], od.seq)

        waited = {e: {} for e in ENGS}
        for o in ops:
            conds = {}
            if o.dma and o.didx >= NDSEM:
                s = dsem[o.eng][o.didx % NDSEM]
                conds[id(s)] = (s, 16 * (o.didx // NDSEM))
            for d in o.deps:
                s, v = done_cond(d)
                if id(s) not in conds or conds[id(s)][1] < v:
                    conds[id(s)] = (s, v)
            wl = waited[o.eng]
            for sid, (s, v) in conds.items():
                if wl.get(sid, 0) < v:
                    wl[sid] = v
                    o.waits.append((s, v))
        final_waits = []
        for e in ENGS:
            n = dcnt[e]
            for j in range(NDSEM):
                cnt = (n - j + NDSEM - 1) // NDSEM if n > j else 0
                if cnt > 0:
                    final_waits.append((dsem[e][j], 16 * cnt))
        byeng = {e: [o for o in ops if o.eng == e] for e in ENGS}
        self.n_inst = {e: len(byeng[e]) for e in ENGS}

        def emit(eng_name):
            def f(eng):
                for o in byeng[eng_name]:
                    for s, v in o.waits:
                        eng.wait_ge(s, v)
                    ins = o.fn(eng)
                    if o.dma:
                        ins.then_inc(dsem[o.eng][o.didx % NDSEM], 16)
                    elif o.signal:
                        ins.then_inc(csem[o.eng], 1)
                if eng_name == "sync":
                    for s, v in final_waits:
                        eng.wait_ge(s, v)
            return f

        with nc.Block() as block:
            block.sync(emit("sync"))
            block.tensor(emit("tensor"))
            block.vector(emit("vector"))
            block.scalar(emit("scalar"))
            block.gpsimd(emit("gpsimd"))
        self.stack.close()
        return nc


D = 2048
ALPHA = float(4 ** 0.25)
EPS = 1e-5
NEG_BIG = -1.0e30
CAP = 512


class Rot:
    def __init__(self, P, name, shape, dt, n, psum=False):
        self.i = 0
        self.name = name
        if psum:
            if not hasattr(P, "banks"):
                P.banks = [P.ps("bank%d" % i, [128, 512], F32) for i in range(8)]
                P.bank_next = 0
            idx = [(P.bank_next + i) % 8 for i in range(n)]
            P.bank_next = (P.bank_next + n) % 8
            if dt == BF16:
                self.t = [P.banks[i][:, :].bitcast(BF16) for i in idx]
            else:
                self.t = [P.banks[i] for i in idx]
            self.keys = [("bank", i) for i in idx]
        else:
            self.t = [P.sb("%s%d" % (name, i), shape, dt) for i in range(n)]
            self.keys = [(name, j) for j in range(n)]

    def next(self):
        j = self.i % len(self.t)
        self.i += 1
        return self.t[j], self.keys[j]


def new_nc():
    return bass.Bass("TRN2", target_bir_lowering=False)


def din(nc, name, shape, dt=F32):
    return nc.dram_tensor(name, list(shape), dt, kind="ExternalInput").ap()


def dout(nc, name, shape, dt=F32):
    return nc.dram_tensor(name, list(shape), dt, kind="ExternalOutput").ap()


def dscr(nc, name, shape, dt):
    return nc.dram_tensor(name, list(shape), dt, kind="Internal").ap()


def setup_consts(P):
    C = {}
    idf = P.sb("ident_f", [128, 128], F32)
    P.memset("gpsimd", idf[:], 0.0, writes=["ident_f"])
    P.op("gpsimd", lambda e: e.affine_select(idf[:], idf[:], [[-1, 128]], ALU.not_equal, 1.0, base=0, channel_multiplier=1),
         reads=["ident_f"], writes=["ident_f"])
    idb = P.sb("ident_b", [128, 128], BF16)
    P.copy("vector", idb[:], idf[:], reads=["ident_f"], writes=["ident_b"])
    onf = P.sb("ones_f", [128, 128], F32)
    P.memset("vector", onf[:], 1.0, writes=["ones_f"])
    onb = P.sb("ones_b", [128, 128], BF16)
    P.memset("vector", onb[:], 1.0, writes=["ones_b"])
    eps = P.sb("eps", [128, 1], F32)
    P.memset("vector", eps[:], EPS, writes=["eps"])
    C.update(ident_f=idf, ident_b=idb, ones_f=onf, ones_b=onb, eps=eps)
    return C


def load_bcast(P, name, src_row, W):
    t = P.sb(name, [128, W], F32)
    P.dma("sync", t[:], src_row.partition_broadcast(128), writes=[name])
    return t


def load_w_bf(P, name, w, K, N, eng="gpsimd"):
    kc = K // 128
    t = P.sb(name, [128, kc, N], BF16)
    wv = w.rearrange("(kc p) n -> p kc n", p=128)
    if not hasattr(P, "wstg"):
        P.wstg = Rot(P, "wstg", [128, 2048], F32, 3)
        P.wcnt = 0
    keys = []
    ces = ["scalar", "vector", "scalar", "gpsimd"]
    for c0 in range(0, N, 512):
        cw = min(512, N - c0)
        kstep = max(1, min(kc, 2048 // cw))
        for k0 in range(0, kc, kstep):
            kn = min(kstep, kc - k0)
            stg, stgk = P.wstg.next()
            sv = stg[:, 0:kn * cw].rearrange("p (k n) -> p k n", n=cw)
            P.dma("sync" if P.wcnt % 2 == 0 else "scalar", sv, wv[:, k0:k0 + kn, c0:c0 + cw], writes=[stgk])
            P.copy(ces[P.wcnt % len(ces)], t[:, k0:k0 + kn, c0:c0 + cw], sv, reads=[stgk], writes=[(name, c0, k0)])
            P.wcnt += 1
            keys.append((name, c0, k0))
    return t, keys


def load_w_bf_sw(P, name, w, K, N):
    kc = K // 128
    t = P.sb(name, [128, kc, N], BF16)
    wv = w.rearrange("(kc p) n -> p kc n", p=128)
    step = max(1, kc // 4)
    for k0 in range(0, kc, step):
        P.dma("gpsimd", t[:, k0:k0 + step, :], wv[:, k0:k0 + step, :], writes=[(name, k0)])
    return t, [(name, k0) for k0 in range(0, kc, step)]


class Prefetch:
    def __init__(self, n, issue):
        self.n, self.issue, self.got = n, issue, {}

    def get(self, i):
        for j in (i, i + 1):
            if j < self.n and j not in self.got:
                self.got[j] = self.issue(j)
        return self.got.pop(i)


class LN:
    def __init__(self, P, C, W, g_row, b_row, name):
        self.P, self.C, self.W, self.name = P, C, W, name
        self.gt = load_bcast(P, name + "_g", g_row, W)
        self.bt = load_bcast(P, name + "_b", b_row, W)
        self.nch = W // 512
        self.st = P.sb(name + "_st", [128, self.nch, 6], F32)
        self.mv = P.sb(name + "_mv", [128, 2], F32)
        self.rs = P.sb(name + "_rs", [128, 1], F32)


def _ln_emit_multi(P, ln, z, zkeys):
    n = ln.name
    for c in range(ln.nch):
        P.op("vector", lambda e, c=c: e.bn_stats(ln.st[:, c, :], z[:, c * 512:(c + 1) * 512]),
             reads=list(zkeys), writes=[(n, "st", c)])
    P.op("vector", lambda e: e.bn_aggr(ln.mv[:, 0:2], ln.st[:].rearrange("p c s -> p (c s)")),
         reads=[(n, "st", c) for c in range(ln.nch)], writes=[(n, "mv")])
    P.act(ln.rs[:], ln.mv[:, 1:2], AF.Sqrt, bias=ln.C["eps"][:], scale=1.0, reads=[(n, "mv"), "eps"], writes=[(n, "rs")])
    P.recip(ln.rs[:], ln.rs[:], reads=[(n, "rs")], writes=[(n, "rs")])
    P.ts("vector", z[:], z[:], ln.mv[:, 0:1], ln.rs[:, 0:1], ALU.subtract, ALU.mult,
         reads=list(zkeys) + [(n, "mv"), (n, "rs")], writes=list(zkeys))
    P.tt("gpsimd", z[:], z[:], ln.gt[:], ALU.mult, reads=list(zkeys) + [n + "_g"], writes=list(zkeys))
    P.tt("gpsimd", z[:], z[:], ln.bt[:], ALU.add, reads=list(zkeys) + [n + "_b"], writes=list(zkeys))


def build_outproj(T):
    nc = new_nc()
    mixT = din(nc, "mixT", [D, T], BF16)
    xres = din(nc, "xres", [T, D])
    w = din(nc, "w", [D, D])
    g = din(nc, "g", [1, D])
    b = din(nc, "b", [1, D])
    y = dout(nc, "y", [T, D])
    yb = dout(nc, "yb", [T, D], BF16)
    P = Prog(nc)
    C = setup_consts(P)
    ln = LN(P, C, D, g, b, "ln")
    zbr = Rot(P, "zb", [128, D], BF16, 2)
    w_bf, wk = load_w_bf(P, "w_bf", w, D, D)
    mr = Rot(P, "mix", [128, 16, 512], BF16, 2)
    psr = Rot(P, "ps", [128, 512], F32, 4, psum=True)
    zr = Rot(P, "z", [128, D], F32, 2)
    xr = Rot(P, "xr", [128, D], F32, 3)
    mv = mixT.rearrange("(kc p) t -> p kc t", p=128)

    def _ld_x(i):
        xt, xk = xr.next()
        P.dma("sync", xt[:], xres[i * 128:(i + 1) * 128, :], writes=[xk])
        return xt, xk

    def _ld_m(tc):
        m, mk = mr.next()
        P.dma("sync", m[:], mv[:, :, tc * 512:(tc + 1) * 512], writes=[mk])
        return m, mk
    xpf = Prefetch(T // 128, _ld_x)
    mpf = Prefetch(T // 512, _ld_m)
    for tc in range(T // 512):
        m, mk = mpf.get(tc)
        for tt in range(4):
            r0 = tc * 512 + tt * 128
            emit_proj_res_ln_multi(P, ln, m, [mk], tt, 16, w_bf, wk, xpf.get(tc * 4 + tt), y[r0:r0 + 128, :], psr, zr, xr, (tc, tt), yb[r0:r0 + 128, :], zbr)
    P.build()
    return nc


def build_cross(T):
    nc = new_nc()
    xT = din(nc, "xT", [D, T], BF16)
    xres = din(nc, "xres", [T, D])
    memT = din(nc, "memT", [D, 256])
    kvw = din(nc, "kvw", [D, 1024])
    xqw = din(nc, "xqw", [D, 512])
    xow = din(nc, "xow", [512, D])
    g = din(nc, "g", [1, D])
    b = din(nc, "b", [1, D])
    y = dout(nc, "y", [T, D])
    yb = dout(nc, "yb", [T, D], BF16)
    P = Prog(nc)
    C = setup_consts(P)
    ln = LN(P, C, D, g, b, "ln")
    zbr = Rot(P, "zb", [128, D], BF16, 2)
    xq_bf, xqk = load_w_bf(P, "xq_bf", xqw, D, 512)
    xo_bf, xok = load_w_bf(P, "xo_bf", xow, 512, D)
    psr = Rot(P, "ps", [128, 512], F32, 4, psum=True)
    pso = Rot(P, "pso", [128, 512], F32, 2, psum=True)
    psl = Rot(P, "psl", [128, 512], F32, 2, psum=True)
    kmT = P.sb("kmT", [128, 4, 256], BF16)
    vm = P.sb("vm", [128, 2, 512], BF16)
    with P.scope():
        kvw_bf, kvk = load_w_bf(P, "kvw_bf", kvw, D, 1024)
        memT_bf, mk_ = load_w_bf(P, "memT_bf", memT, D, 256)
        for h in range(4):
            ps, pk = psr.next()
            for kc in range(16):
                P.mm(ps[:, 0:256], kvw_bf[:, kc, h * 128:(h + 1) * 128], memT_bf[:, kc, :], start=(kc == 0), stop=(kc == 15),
                     reads=kvk + mk_, writes=[pk])
            P.copy("vector", kmT[:, h, :], ps[:, 0:256], reads=[pk], writes=[("kmT", h)])
        for mt in range(2):
            ps, pk = psr.next()
            for kc in range(16):
                P.mm(ps[:, :], memT_bf[:, kc, mt * 128:(mt + 1) * 128], kvw_bf[:, kc, 512:1024], start=(kc == 0), stop=(kc == 15),
                     reads=kvk + mk_, writes=[pk])
            P.copy("vector", vm[:, mt, :], ps[:, :], reads=[pk], writes=[("vm", mt)])
    kmk = [("kmT", h) for h in range(4)]
    vmk = [("vm", mt) for mt in range(2)]
    xr_ = Rot(P, "xTc", [128, 16, 512], BF16, 2)
    qr = Rot(P, "qT", [128, 512], BF16, 4)
    pr = Rot(P, "pT", [128, 512], BF16, 8)
    rlr = Rot(P, "rL", [128, 512], F32, 2)
    otr = Rot(P, "oT", [128, 4, 512], BF16, 2)
    zr = Rot(P, "z", [128, D], F32, 2)
    xr = Rot(P, "xr", [128, D], F32, 3)
    xv = xT.rearrange("(kc p) t -> p kc t", p=128)
    scale = float(128 ** -0.5)

    def _ld_x(i):
        xt, xk_ = xr.next()
        P.dma("sync", xt[:], xres[i * 128:(i + 1) * 128, :], writes=[xk_])
        return xt, xk_
    xpf = Prefetch(T // 128, _ld_x)
    for tc in range(T // 512):
        xc, xk = xr_.next()
        for q4 in range(4):
            P.dma("sync", xc[:, q4 * 4:(q4 + 1) * 4, :], xv[:, q4 * 4:(q4 + 1) * 4, tc * 512:(tc + 1) * 512], writes=[(xk, q4)])
        xks = [(xk, q4) for q4 in range(4)]
        oT, ok = otr.next()
        qs = []
        for h in range(4):
            ps, pk = psr.next()
            for kc in range(16):
                P.mm(ps[:, :], xq_bf[:, kc, h * 128:(h + 1) * 128], xc[:, kc, :], start=(kc == 0), stop=(kc == 15),
                     reads=xqk + xks, writes=[pk])
            q, qk = qr.next()
            P.copy("scalar", q[:], ps[:, :], reads=[pk], writes=[qk])
            qs.append((q, qk))
        ptsl = []
        for h in range(4):
            q, qk = qs[h]
            pts = []
            for mt in range(2):
                ps2, pk2 = psr.next()
                P.mm(ps2[:, :], kmT[:, h, mt * 128:(mt + 1) * 128], q[:], reads=kmk + [qk], writes=[pk2])
                pt, ptk = pr.next()
                P.act(pt[:], ps2[:, :], AF.Exp, scale=scale, reads=[pk2], writes=[ptk])
                pts.append((pt, ptk))
            ptsl.append(pts)
        for h in range(4):
            pts = ptsl[h]
            po, pok = pso.next()
            pl, plk = psl.next()
            for mt in range(2):
                P.mm(po[:, :], vm[:, mt, h * 128:(h + 1) * 128], pts[mt][0][:], start=(mt == 0), stop=(mt == 1),
                     reads=vmk + [pts[mt][1]], writes=[pok])
            for mt in range(2):
                P.mm(pl[:, :], C["ones_b"][:], pts[mt][0][:], start=(mt == 0), stop=(mt == 1),
                     reads=["ones_b", pts[mt][1]], writes=[plk])
            rl, rlk = rlr.next()
            P.recip(rl[:], pl[:, :], reads=[plk], writes=[rlk])
            P.tt("vector", oT[:, h, :], po[:, :], rl[:], ALU.mult, reads=[pok, rlk], writes=[(ok, h)])
        oks = [(ok, h) for h in range(4)]
        for tt in range(4):
            r0 = tc * 512 + tt * 128
            emit_proj_res_ln_multi(P, ln, oT, oks, tt, 4, xo_bf, xok, xpf.get(tc * 4 + tt), y[r0:r0 + 128, :], psr, zr, xr, (tc, tt), yb[r0:r0 + 128, :], zbr)
    P.build()
    return nc


def emit_proj_res_ln_multi(P, ln, lhs, lhsks, tt, KC, w_bf, wkeys, xres_ap, out_ap, psr, zr, xr, ntag, outb_ap=None, zbr=None):
    xt, xk = xres_ap
    z, zk = zr.next()
    for cg in range(4):
        ps, pk = psr.next()
        for kc in range(KC):
            P.mm(ps[:, :], lhs[:, kc, tt * 128:(tt + 1) * 128], w_bf[:, kc, cg * 512:(cg + 1) * 512],
                 start=(kc == 0), stop=(kc == KC - 1), reads=list(lhsks) + list(wkeys), writes=[pk])
        P.stt("vector", z[:, cg * 512:(cg + 1) * 512], xt[:, cg * 512:(cg + 1) * 512], ALPHA, ps[:, :], ALU.mult, ALU.add,
              reads=[xk, pk], writes=[(zk, cg)])
    zks = [(zk, cg) for cg in range(4)]
    _ln_emit_multi(P, ln, z, zks)
    P.dma("sync", out_ap, z[:], reads=zks, writes=[("out", ntag)])
    if outb_ap is not None:
        zb, zbk = zbr.next()
        P.copy("scalar", zb[:], z[:], reads=zks, writes=[zbk])
        P.dma("sync", outb_ap, zb[:], reads=[zbk], writes=[("outb", ntag)])


def build_moe(T, debug=False):
    NT = T // 128
    NS = CAP // 128
    nc = new_nc()
    xT = din(nc, "xT", [D, T])
    xres = din(nc, "xres", [T, D])
    rw = din(nc, "rw", [D, 36])
    rb = din(nc, "rb", [1, 36])
    ecap = din(nc, "ecap", [1, 32])
    w1 = din(nc, "w1", [32, D, 512])
    w3 = din(nc, "w3", [32, D, 512])
    w2 = din(nc, "w2", [32, 512, D])
    g = din(nc, "g", [1, D])
    b = din(nc, "b", [1, D])
    y = dout(nc, "y", [T, D])
    yb = dout(nc, "yb", [T, D], BF16)
    cnt_o = dout(nc, "cnt", [1, 32], F32)
    Xg = dscr(nc, "Xg", [32 * CAP, D], BF16)
    Yg = dscr(nc, "Yg", [32 * CAP, D], F32)
    P = Prog(nc)
    C = setup_consts(P)
    _bc = {}

    def _bcreg(e):
        if "r" not in _bc:
            _bc["r"] = e.to_reg(32 * CAP - 1)
        return _bc["r"]
    dest_i = P.sb("dest_i", [128, NT, 2], I32)
    gate = P.sb("gate", [128, NT, 2], F32)
    uf = P.sb("uf", [128, 128], F32)
    P.memset("gpsimd", uf[:], 1.0, writes=["uf"])
    P.op("gpsimd", lambda e: e.affine_select(uf[:], uf[:], [[1, 128]], ALU.is_ge, 0.0, base=-1, channel_multiplier=-1),
         reads=["uf"], writes=["uf"])
    ub = P.sb("ub", [128, 128], BF16)
    P.copy("vector", ub[:], uf[:], reads=["uf"], writes=["ub"])

    with P.scope():
        zt = P.sb("zt", [128, 8192], BF16)
        P.memset("gpsimd", zt[:], 0.0, writes=["zt"])
        xgz = Xg.rearrange("(n p f) d -> n p (f d)", p=128, f=4)
        assert (32 * CAP) % 512 == 0
        zkeys = []
        for i in range(32 * CAP // 512):
            P.dma("sync", xgz[i], zt[:], reads=["zt"], writes=[("xg0", i)])
            zkeys.append(("xg0", i))
        rw_f = P.sb("rw_f", [128, 16, 36], F32)
        P.dma("sync", rw_f[:], rw.rearrange("(kc p) n -> p kc n", p=128), writes=["rw_f"])
        rbt = load_bcast(P, "rbt", rb, 36)
        ect = load_bcast(P, "ect", ecap, 32)
        base = P.sb("base", [128, 32], F32)
        P.memset("vector", base[:], 0.0, writes=["base"])
        xcr = Rot(P, "xTf", [128, 16, 512], F32, 2)
        xrr = Rot(P, "xrt", [128, D], F32, 2)
        xbr = Rot(P, "xb", [128, D], BF16, 2)
        psr = Rot(P, "psr", [128, 512], F32, 2, psum=True)
        psq = Rot(P, "psq", [128, 512], F32, 2, psum=True)
        sm = {}
        for nm, w_, dt_ in [("lg", 36, F32), ("gmax", 1, F32), ("ngmax", 1, F32), ("gsel", 4, F32), ("gexp", 4, F32), ("gsum", 1, F32),
                            ("ggate", 1, F32), ("pen", 4, F32), ("em", 32, F32), ("top8", 8, F32), ("sel1", 32, F32), ("sel2", 32, F32),
                            ("dd", 1, F32), ("ee", 1, F32), ("den", 1, F32), ("p1", 1, F32), ("p2", 1, F32), ("A", 32, BF16),
                            ("slot", 32, F32), ("tmp", 32, F32), ("destf", 2, F32)]:
            sm[nm] = P.sb("r_" + nm, [128, w_], dt_)
        xv = xT.rearrange("(kc p) t -> p kc t", p=128)
        for tc in range(T // 512):
            xc, xk = xcr.next()
            for q4 in range(4):
                P.dma("sync", xc[:, q4 * 4:(q4 + 1) * 4, :], xv[:, q4 * 4:(q4 + 1) * 4, tc * 512:(tc + 1) * 512], writes=[(xk, q4)])
            xks = [(xk, q4) for q4 in range(4)]
            for tt in range(4):
                ti = tc * 4 + tt
                r0 = ti * 128
                ps, pk = psr.next()
                for kc in range(16):
                    P.mm(ps[:, 0:36], xc[:, kc, tt * 128:(tt + 1) * 128], rw_f[:, kc, :], start=(kc == 0), stop=(kc == 15),
                         reads=xks + ["rw_f"], writes=[pk])
                V = "vector"
                P.tt(V, sm["lg"][:], ps[:, 0:36], rbt[:], ALU.add, reads=[pk, "rbt"], writes=["lg"])
                P.red(V, sm["gmax"][:], sm["lg"][:, 0:4], ALU.max, reads=["lg"], writes=["gmax"])
                P.ts(V, sm["gsel"][:], sm["lg"][:, 0:4], sm["gmax"][:, 0:1], None, ALU.is_equal, reads=["lg", "gmax"], writes=["gsel"])
                P.ts(V, sm["ngmax"][:], sm["gmax"][:], -1.0, None, ALU.mult, reads=["gmax"], writes=["ngmax"])
                P.memset(V, sm["gsum"][:], 0.0, writes=["gsum"])
                P.act(sm["gexp"][:], sm["lg"][:, 0:4], AF.Exp, bias=sm["ngmax"][:], scale=1.0, accum_out=sm["gsum"][:],
                      reads=["lg", "ngmax", "gsum"], writes=["gexp", "gsum"])
                P.recip(sm["ggate"][:], sm["gsum"][:], reads=["gsum"], writes=["ggate"])
                P.ts(V, sm["pen"][:], sm["gsel"][:], -1.0, 1.0e30, ALU.add, ALU.mult, reads=["gsel"], writes=["pen"])
                for gi in range(4):
                    P.ts(V, sm["em"][:, gi * 8:(gi + 1) * 8], sm["lg"][:, 4 + gi * 8:4 + (gi + 1) * 8], sm["pen"][:, gi:gi + 1], None, ALU.add,
                         reads=["lg", "pen"], writes=[("em", gi)])
                emk = [("em", gi) for gi in range(4)]
                P.op(V, lambda e: e.max(sm["top8"][:], sm["em"][:]), reads=emk, writes=["top8"])
                P.ts(V, sm["sel1"][:], sm["em"][:], sm["top8"][:, 0:1], None, ALU.is_equal, reads=emk + ["top8"], writes=["sel1"])
                P.ts(V, sm["sel2"][:], sm["em"][:], sm["top8"][:, 1:2], None, ALU.is_equal, reads=emk + ["top8"], writes=["sel2"])
                P.tt(V, sm["dd"][:], sm["top8"][:, 1:2], sm["top8"][:, 0:1], ALU.subtract, reads=["top8"], writes=["dd"])
                P.act(sm["ee"][:], sm["dd"][:], AF.Exp, reads=["dd"], writes=["ee"])
                P.ts(V, sm["den"][:], sm["ee"][:], 1.0, None, ALU.add, reads=["ee"], writes=["den"])
                P.recip(sm["p1"][:], sm["den"][:], reads=["den"], writes=["p1"])
                P.tt(V, sm["p2"][:], sm["ee"][:], sm["p1"][:], ALU.mult, reads=["ee", "p1"], writes=["p2"])
                P.tt(V, gate[:, ti, 0:1], sm["p1"][:], sm["ggate"][:], ALU.mult, reads=["p1", "ggate"], writes=[("gate", ti, 0)])
                P.tt(V, gate[:, ti, 1:2], sm["p2"][:], sm["ggate"][:], ALU.mult, reads=["p2", "ggate"], writes=[("gate", ti, 1)])
                P.tt(V, sm["A"][:], sm["sel1"][:], sm["sel2"][:], ALU.add, reads=["sel1", "sel2"], writes=["A"])
                pq, pqk = psq.next()
                P.mm(pq[:, 0:32], ub[:], sm["A"][:], reads=["ub", "A"], writes=[(pqk, 0)])
                P.mm(pq[:, 32:64], C["ones_b"][:], sm["A"][:], reads=["ones_b", "A"], writes=[(pqk, 1)])
                P.tt(V, sm["slot"][:], pq[:, 0:32], base[:], ALU.add, reads=[(pqk, 0), "base"], writes=["slot"])
                P.tt(V, sm["slot"][:], sm["slot"][:], ect[:], ALU.add, reads=["slot", "ect"], writes=["slot"])
                P.tt(V, base[:], base[:], pq[:, 32:64], ALU.add, reads=["base", (pqk, 1)], writes=["base"])
                for k, sname in enumerate(("sel1", "sel2")):
                    P.tt(V, sm["tmp"][:], sm[sname][:], sm["slot"][:], ALU.mult, reads=[sname, "slot"], writes=["tmp"])
                    P.red(V, sm["destf"][:, k:k + 1], sm["tmp"][:], ALU.add, reads=["tmp"], writes=[("destf", k)])
                P.copy(V, dest_i[:, ti, :], sm["destf"][:], reads=[("destf", 0), ("destf", 1)], writes=[("dest", ti)])
                xr_t, xrk = xrr.next()
                P.dma("sync", xr_t[:], xres[r0:r0 + 128, :], writes=[xrk])
                xb, xbk = xbr.next()
                P.copy("scalar", xb[:], xr_t[:], reads=[xrk], writes=[xbk])
                for k in range(0 if debug == 1 else 2):
                    P.op("gpsimd", lambda e, xb=xb, ti=ti, k=k: e.indirect_dma_start(
                        out=Xg, out_offset=bass.IndirectOffsetOnAxis(ap=dest_i[:, ti, k:k + 1], axis=0), in_=xb[:], in_offset=None, bounds_check=_bcreg(e), oob_is_err=False),
                        reads=[xbk, ("dest", ti)] + zkeys, writes=[("xg", ti, k)], dma=True)
        P.dma("sync", cnt_o, base[0:1, :], reads=["base"], writes=["cnt_o"])
    xgkeys = [("xg", ti, k) for ti in range(NT) for k in range(2)]
    def dbg_exit():
        od = dout(nc, "o_dest", [128, NT, 2], I32)
        og = dout(nc, "o_gate", [128, NT, 2], F32)
        P.dma("sync", od, dest_i[:], reads=[("dest", ti) for ti in range(NT)])
        P.dma("sync", og, gate[:], reads=[("gate", ti, k) for ti in range(NT) for k in range(2)])
        P.barrier()
        P.build()
        return nc
    if debug in (1, 3):
        return dbg_exit()

    with P.scope():
        w1r = Rot(P, "w1b", [128, 16, 512], BF16, 2)
        w3r = Rot(P, "w3b", [128, 16, 512], BF16, 2)
        w2r = Rot(P, "w2b", [128, 4, D], BF16, 2)
        xgr = Rot(P, "xgr", [128, NS, D], BF16, 2)
        stgr = Rot(P, "wstg", [128, 2048], F32, 3)
        dcount = [0]
        xgT = P.sb("xgT", [128, 16, CAP], BF16)
        hhr = Rot(P, "hhT", [128, 4, CAP], BF16, 2)
        slr = Rot(P, "sl", [128, CAP], F32, 2)
        yrr = Rot(P, "yrow", [128, D], F32, 2)
        pst = Rot(P, "pst", [128, 512], BF16, 2, psum=True)
        psh = Rot(P, "psh", [128, 512], F32, 4, psum=True)
        psy = Rot(P, "psy", [128, 512], F32, 2, psum=True)
        ykeys = []
        cast_eng = ["gpsimd"]

        def load_expert(e_):
            w1b, w1k = w1r.next()
            w3b, w3k = w3r.next()
            w2b, w2k = w2r.next()
            w2v_ = w2[e_].rearrange("(kc p) n -> p kc n", p=128)
            for h2 in range(2):
                P.dma("gpsimd", w2b[:, h2 * 2:(h2 + 1) * 2, :], w2v_[:, h2 * 2:(h2 + 1) * 2, :], writes=[(w2k, h2)])
            for wi, (wsrc, wdst, wkk, npiece, kcs) in enumerate(((w1, w1b, w1k, 4, 4), (w3, w3b, w3k, 4, 4))):
                wv_ = wsrc[e_].rearrange("(kc p) n -> p kc n", p=128)
                for pc_ in range(npiece):
                    stg, stgk = stgr.next()
                    ncol = 512 if wi < 2 else D
                    sv = stg[:, :].rearrange("p (k n) -> p k n", n=ncol)
                    P.dma("sync", sv, wv_[:, pc_ * kcs:(pc_ + 1) * kcs, :], writes=[stgk])
                    ce = cast_eng[dcount[0] % len(cast_eng)]
                    dcount[0] += 1
                    P.copy(ce, wdst[:, pc_ * kcs:(pc_ + 1) * kcs, :], sv, reads=[stgk], writes=[(wkk, pc_)])
            xg, xgk = xgr.next()
            P.dma("sync", xg[:], Xg[e_ * CAP:(e_ + 1) * CAP, :].rearrange("(s p) d -> p s d", p=128), reads=xgkeys, writes=[xgk])
            return (w1b, [(w1k, q_) for q_ in range(4)], w3b, [(w3k, q_) for q_ in range(4)], w2b, [(w2k, q_) for q_ in range(2)], xg, xgk)

        def compute_expert(e_, ld):
            w1b, w1ks, w3b, w3ks, w2b, w2ks, xg, xgk = ld
            for kc in range(16):
                pt, ptk = pst.next()
                for s_ in range(NS):
                    P.tr(pt[:, s_ * 128:(s_ + 1) * 128], xg[:, s_, kc * 128:(kc + 1) * 128], C["ident_b"][:], reads=[xgk, "ident_b"], writes=[ptk])
                P.copy("vector" if kc % 2 == 0 else "scalar", xgT[:, kc, :], pt[:, 0:CAP], reads=[ptk], writes=[("xgT", kc)])
            xtk = [("xgT", kc) for kc in range(16)]
            hh, hhk = hhr.next()
            for fc in range(4):
                p1_, p1k = psh.next()
                for kc in range(16):
                    P.mm(p1_[:, 0:CAP], w1b[:, kc, fc * 128:(fc + 1) * 128], xgT[:, kc, :], start=(kc == 0), stop=(kc == 15), reads=w1ks + xtk, writes=[p1k])
                p3_, p3k = psh.next()
                for kc in range(16):
                    P.mm(p3_[:, 0:CAP], w3b[:, kc, fc * 128:(fc + 1) * 128], xgT[:, kc, :], start=(kc == 0), stop=(kc == 15), reads=w3ks + xtk, writes=[p3k])
                sl, slk = slr.next()
                P.act(sl[:], p1_[:, 0:CAP], AF.Silu, reads=[p1k], writes=[slk])
                P.tt("vector", hh[:, fc, :], sl[:], p3_[:, 0:CAP], ALU.mult, reads=[slk, p3k], writes=[(hhk, fc)])
            hks = [(hhk, fc) for fc in range(4)]
            for st_ in range(NS):
                yr, yrk = yrr.next()
                for cg in range(4):
                    py, pyk = psy.next()
                    for fc in range(4):
                        P.mm(py[:, :], hh[:, fc, st_ * 128:(st_ + 1) * 128], w2b[:, fc, cg * 512:(cg + 1) * 512], start=(fc == 0), stop=(fc == 3),
                             reads=hks + w2ks, writes=[pyk])
                    P.copy("scalar" if cg % 2 == 0 else "vector", yr[:, cg * 512:(cg + 1) * 512], py[:, :], reads=[pyk], writes=[(yrk, cg)])
                r0 = e_ * CAP + st_ * 128
                P.dma("sync", Yg[r0:r0 + 128, :], yr[:], reads=[(yrk, cg) for cg in range(4)], writes=[("yg", e_, st_)])
                ykeys.append(("yg", e_, st_))

        cur = load_expert(0)
        for e_ in range(32):
            nxt = load_expert(e_ + 1) if e_ + 1 < 32 else None
            compute_expert(e_, cur)
            cur = nxt

    if debug == 2:
        return dbg_exit()
    with P.scope():
        ln = LN(P, C, D, g, b, "ln")
        y1r = Rot(P, "y1", [128, D], F32, 3)
        zbr = Rot(P, "zb", [128, D], BF16, 2)
        y2r = Rot(P, "y2", [128, D], F32, 3)
        xrr = Rot(P, "xrc", [128, D], F32, 3)
        def _ld3(ti):
            y1, y1k = y1r.next()
            y2, y2k = y2r.next()
            for k, (yt, ytk) in enumerate(((y1, y1k), (y2, y2k))):
                P.op("gpsimd", lambda e, yt=yt, ti=ti, k=k: e.indirect_dma_start(
                    out=yt[:], out_offset=None, in_=Yg, in_offset=bass.IndirectOffsetOnAxis(ap=dest_i[:, ti, k:k + 1], axis=0), bounds_check=_bcreg(e), oob_is_err=False),
                    reads=ykeys + [("dest", ti)], writes=[ytk], dma=True)
            xt, xk = xrr.next()
            P.dma("sync", xt[:], xres[ti * 128:(ti + 1) * 128, :], writes=[xk])
            return y1, y1k, y2, y2k, xt, xk
        pf3 = Prefetch(NT, _ld3)
        for ti in range(NT):
            r0 = ti * 128
            y1, y1k, y2, y2k, xt, xk = pf3.get(ti)
            P.act(y1[:], y1[:], AF.Identity, scale=gate[:, ti, 0:1], reads=[y1k, ("gate", ti, 0)], writes=[y1k])
            P.stt("vector", y1[:], y2[:], gate[:, ti, 1:2], y1[:], ALU.mult, ALU.add, reads=[y1k, y2k, ("gate", ti, 1)], writes=[y1k])
            P.stt("vector", y1[:], xt[:], ALPHA, y1[:], ALU.mult, ALU.add, reads=[y1k, xk], writes=[y1k])
            _ln_emit_multi(P, ln, y1, [y1k])
            P.dma("sync", y[r0:r0 + 128, :], y1[:], reads=[y1k], writes=[("out", ti)])
            zb, zbk = zbr.next()
            P.copy("scalar", zb[:], y1[:], reads=[y1k], writes=[zbk])
            P.dma("sync", yb[r0:r0 + 128, :], zb[:], reads=[zbk], writes=[("outb", ti)])
    P.build()
    return nc


LAM_INIT0 = 0.8 - 0.6 * 1.0


def build_attn(S):
    NCH = S // 512
    nc = new_nc()
    xT = din(nc, "xT", [D, S])
    wq = din(nc, "wq", [D, 256])
    wk = din(nc, "wk", [D, 256])
    wv = din(nc, "wv", [D, 256])
    wqp = din(nc, "wqp", [D, 64])
    wkp = din(nc, "wkp", [D, 64])
    pos = din(nc, "pos", [1, S], I32)
    rc = din(nc, "rc", [32, 5])
    lamv = din(nc, "lamv", [1, 512])
    subg = din(nc, "subg", [128, 2])
    oT = dout(nc, "oT", [256, S], BF16)
    P = Prog(nc)
    C = setup_consts(P)
    V_, G_ = "vector", "gpsimd"
    trf = P.sb("trf", [128, 128], F32)
    P.memset(G_, trf[:], 1.0, writes=["trf"])
    P.op(G_, lambda e: e.affine_select(trf[:], trf[:], [[1, 128]], ALU.is_ge, 0.0, base=0, channel_multiplier=-1), reads=["trf"], writes=["trf"])
    tri = P.sb("tri", [128, 128], BF16)
    P.copy(V_, tri[:], trf[:], reads=["trf"], writes=["tri"])
    rct = P.sb("rct", [32, 5], F32)
    P.dma("sync", rct[:], rc, writes=["rct"])
    sgt = P.sb("sgt", [128, 2], F32)
    P.dma("sync", sgt[:], subg, writes=["sgt"])
    P.ts(V_, sgt[:], sgt[:], float(1.0 - LAM_INIT0), None, ALU.mult, reads=["sgt"], writes=["sgt"])
    lv = P.sb("lv", [1, 512], F32)
    P.dma("sync", lv[:], lamv, writes=["lv"])
    lt = P.sb("lt", [1, 8], F32)
    P.tt(V_, lv[:, 0:128], lv[:, 0:128], lv[:, 128:256], ALU.mult, reads=["lv"], writes=["lv"])
    P.tt(V_, lv[:, 256:384], lv[:, 256:384], lv[:, 384:512], ALU.mult, reads=["lv"], writes=["lv"])
    P.red(V_, lt[:, 0:1], lv[:, 0:128], ALU.add, reads=["lv"], writes=["lt0"])
    P.red(V_, lt[:, 1:2], lv[:, 256:384], ALU.add, reads=["lv"], writes=["lt1"])
    P.act(lt[:, 2:4], lt[:, 0:2], AF.Exp, reads=["lt0", "lt1"], writes=["lt2"])
    P.tt(V_, lt[:, 4:5], lt[:, 3:4], lt[:, 2:3], ALU.subtract, reads=["lt2"], writes=["lt4"])
    P.ts(V_, lt[:, 5:6], lt[:, 4:5], float(-LAM_INIT0), None, ALU.add, reads=["lt4"], writes=["lt5"])
    sbk = Rot(P, "psS", [128, 512], F32, 5, psum=True)
    obk = Rot(P, "psO", [128, 512], F32, 2, psum=True)
    lbk = Rot(P, "psL", [128, 512], F32, 1, psum=True)
    nlam = P.sb("nlam", [128, 1], F32)
    ps, pk = sbk.next()
    P.mm(ps[:, 0:1], C["ones_f"][0:1, :], lt[0:1, 5:6], reads=["ones_f", "lt5"], writes=[pk])
    P.copy(V_, nlam[:], ps[:, 0:1], reads=[pk], writes=["nlam"])
    wq_bf, wqk = load_w_bf_sw(P, "wq_bf", wq, D, 256)
    wk_bf, wkk = load_w_bf_sw(P, "wk_bf", wk, D, 256)
    wv_bf, wvk = load_w_bf_sw(P, "wv_bf", wv, D, 256)
    wqp_bf, wqpk = load_w_bf_sw(P, "wqp_bf", wqp, D, 64)
    wkp_bf, wkpk = load_w_bf_sw(P, "wkp_bf", wkp, D, 64)
    Kc = P.sb("Kc", [128, 2, S], BF16)
    Vc = P.sb("Vc", [128, S // 128, 256], BF16)
    xTc = P.sb("xTc", [128, 16, 256], BF16)
    qT = P.sb("qT", [128, 2, 512], BF16)
    posi = P.sb("posi", [32, 512], I32)
    posf = P.sb("posf", [32, 512], F32)
    frs = P.sb("frs", [32, 512], F32)
    frsi = posi
    frsf = P.sb("frsf", [32, 512], F32)
    cosT = P.sb("cosT", [32, 512], F32)
    sinT = P.sb("sinT", [32, 512], F32)
    rt1 = Rot(P, "rt1", [32, 256], F32, 1)
    rt2 = Rot(P, "rt2", [32, 256], F32, 1)
    ptr = Rot(P, "pT", [128, 512], BF16, 6)
    rL = P.sb("rL", [128, 512], F32)
    om = [P.sb("om0", [128, 2, 512], F32), P.sb("om1", [128, 2, 512], F32)]
    rstd = P.sb("rstd", [128, 512], F32)
    obf = Rot(P, "obf", [128, 2, 512], BF16, 1)
    xv = xT.rearrange("(kc p) t -> p kc t", p=128)
    oTv = oT.rearrange("(c p) t -> p c t", p=128)
    scale = float(128 ** -0.5)
    for i in range(NCH):
        c0 = i * 512
        P.dma("sync", posi[:], pos[0:1, c0:c0 + 512].partition_broadcast(32), writes=["posi"])
        P.copy(V_, posf[:], posi[:], reads=["posi"], writes=["posf"])
        for which_tab, (tab, shift, scol) in enumerate(((sinT, 0.0, 1), (cosT, 0.25, 2))):
            P.ts(V_, frs[:], posf[:], rct[:, 0:1], shift, ALU.mult, ALU.add, reads=["posf", "rct"], writes=["frs"])
            P.copy(V_, frsi[:], frs[:], reads=["frs", "posf"], writes=["posi"])
            P.copy(V_, frsf[:], frsi[:], reads=["posi"], writes=["frsf"])
            P.tt(V_, frs[:], frs[:], frsf[:], ALU.subtract, reads=["frs", "frsf"], writes=["frs"])
            P.ts(V_, frsf[:], frs[:], 0.5, None, ALU.is_gt, reads=["frs"], writes=["frsf"])
            P.tt(V_, frs[:], frs[:], frsf[:], ALU.subtract, reads=["frs", "frsf"], writes=["frs"])
            P.act(tab[:], frs[:], AF.Sin, scale=rct[:, scol:scol + 1], reads=["frs", "rct"], writes=["sinT" if which_tab == 0 else "cosT"])
        for hf in range(2):
            h0 = hf * 256
            for q4 in range(4):
                P.dma("gpsimd", xTc[:, q4 * 4:(q4 + 1) * 4, :], xv[:, q4 * 4:(q4 + 1) * 4, c0 + h0:c0 + h0 + 256], writes=[("xTc", q4)])
            xks = [("xTc", q4) for q4 in range(4)]
            for which in range(2):
                w_bf, wks, wp_bf, wpks = (wq_bf, wqk, wqp_bf, wqpk) if which == 0 else (wk_bf, wkk, wkp_bf, wkpk)
                for m in range(2):
                    dst = qT[:, m, h0:h0 + 256] if which == 0 else Kc[:, m, c0 + h0:c0 + h0 + 256]
                    dk = ("qT", m, hf) if which == 0 else ("Kc", m, i, hf)
                    pm, pmk = sbk.next()
                    for kc in range(16):
                        P.mm(pm[:, 0:256], w_bf[:, kc, m * 128:(m + 1) * 128], xTc[:, kc, :], start=(kc == 0), stop=(kc == 15), reads=wks + xks, writes=[pmk])
                    pp, ppk = sbk.next()
                    for kc in range(16):
                        P.mm(pp[0:32, 0:256], wp_bf[:, kc, m * 32:(m + 1) * 32], xTc[:, kc, :], start=(kc == 0), stop=(kc == 15), reads=wpks + xks, writes=[ppk])
                    P.copy("scalar", dst[32:64, :], pm[32:64, 0:256], reads=[pmk], writes=[(dk, "hi0")])
                    P.copy("scalar", dst[64:128, :], pm[64:128, 0:256], reads=[pmk], writes=[(dk, "hi")])
                    t1, t1k = rt1.next()
                    t2, t2k = rt2.next()
                    P.tt(V_, t1[:], pm[0:32, 0:256], cosT[:, h0:h0 + 256], ALU.mult, reads=[pmk, "cosT"], writes=[t1k])
                    P.tt(V_, t2[:], pp[0:32, 0:256], sinT[:, h0:h0 + 256], ALU.mult, reads=[ppk, "sinT"], writes=[t2k])
                    P.tt(G_, dst[0:32, :], t1[:], t2[:], ALU.add, reads=[t1k, t2k], writes=[(dk, "lo")])
            for t2_ in range(2):
                tt = hf * 2 + t2_
                pv, pvk = sbk.next()
                for kc in range(16):
                    P.mm(pv[:, 0:256], xTc[:, kc, t2_ * 128:(t2_ + 1) * 128], wv_bf[:, kc, :], start=(kc == 0), stop=(kc == 15), reads=wvk + xks, writes=[pvk])
                P.copy("scalar" if tt % 2 == 0 else V_, Vc[:, i * 4 + tt, :], pv[:, 0:256], reads=[pvk], writes=[("Vc", i * 4 + tt)])
        jlist = [(4 * i + o, o) for o in range(4)] + [(j, -1) for j in range(4 * i)]
        LA = 3
        for m in range(2):
            Ob = [obk.next() for dvc in range(2)]
            lb, lbk_ = lbk.next()
            pend = []

            def emit_qk(j, o, m=m):
                n0 = o * 128 if o > 0 else 0
                jc = j // 4
                ps, pk = sbk.next()
                P.mm(ps[:, n0:512], Kc[:, m, j * 128:(j + 1) * 128], qT[:, m, n0:512],
                     reads=[(("Kc", m, jc, hf_), part) for hf_ in range(2) for part in ("hi", "hi0", "lo")]
                     + [(("qT", m, hf_), part) for hf_ in range(2) for part in ("hi", "hi0", "lo")], writes=[pk])
                pt, ptk = ptr.next()
                P.act(pt[:, n0:512], ps[:, n0:512], AF.Exp, scale=scale, reads=[pk], writes=[ptk])
                if o >= 0:
                    P.tt(G_, pt[:, n0:n0 + 128], pt[:, n0:n0 + 128], tri[:], ALU.mult, reads=[ptk, "tri"], writes=[ptk])
                return (j, n0, pt, ptk)

            def emit_pv(item, first, last, Ob=Ob, lb=lb, lbk_=lbk_):
                j, n0, pt, ptk = item
                for dvc in range(2):
                    ob, obk_ = Ob[dvc]
                    P.mm(ob[:, n0:512], Vc[:, j, dvc * 128:(dvc + 1) * 128], pt[:, n0:512], start=first, stop=last,
                         reads=[("Vc", j), ptk], writes=[obk_], skip_group_check=True)
                P.mm(lb[:, n0:512], C["ones_b"][:], pt[:, n0:512], start=first, stop=last, reads=["ones_b", ptk], writes=[lbk_], skip_group_check=True)

            ndone = 0
            for (j, o) in jlist:
                pend.append(emit_qk(j, o))
                if len(pend) > LA:
                    emit_pv(pend.pop(0), ndone == 0, False)
                    ndone += 1
            while pend:
                emit_pv(pend.pop(0), ndone == 0, len(pend) == 0)
                ndone += 1
            P.recip(rL[:], lb[:, :], reads=[lbk_], writes=["rL"])
            for dvc in range(2):
                P.tt(V_, om[m][:, dvc, :], Ob[dvc][0][:, :], rL[:], ALU.mult, reads=[Ob[dvc][1], "rL"], writes=[("om", m, dvc)])
        for dvc in range(2):
            P.stt(V_, om[1][:, dvc, :], om[1][:, dvc, :], nlam[:, 0:1], om[0][:, dvc, :], ALU.mult, ALU.add,
                  reads=[("om", 0, dvc), ("om", 1, dvc), "nlam"], writes=[("om", 1, dvc)])
        for dvc in range(2):
            P.act(om[0][:, dvc, :], om[1][:, dvc, :], AF.Square, reads=[("om", 1, dvc)], writes=[("om", 0, dvc)])
        ps, pk = sbk.next()
        for dvc in range(2):
            P.mm(ps[:, :], C["ones_f"][:], om[0][:, dvc, :], start=(dvc == 0), stop=(dvc == 1), reads=["ones_f", ("om", 0, dvc)], writes=[pk])
        P.act(rstd[:], ps[:, :], AF.Sqrt, scale=float(1.0 / 256.0), bias=C["eps"][:], reads=[pk, "eps"], writes=["rstd"])
        P.recip(rstd[:], rstd[:], reads=["rstd"], writes=["rstd"])
        ob_, obfk = obf.next()
        for dvc in range(2):
            P.tt(G_, om[1][:, dvc, :], om[1][:, dvc, :], rstd[:], ALU.mult, reads=[("om", 1, dvc), "rstd"], writes=[("om", 1, dvc)])
            P.act(ob_[:, dvc, :], om[1][:, dvc, :], AF.Identity, scale=sgt[:, dvc:dvc + 1], reads=[("om", 1, dvc), "sgt"], writes=[(obfk, dvc)])
        P.dma("sync", oTv[:, :, c0:c0 + 512], ob_[:], reads=[(obfk, 0), (obfk, 1)], writes=[("oT", i)])
    P.build()
    return nc


def build_conf(T):
    HAL = 128
    nc = new_nc()
    xTh = din(nc, "xTh", [D, HAL + T])
    wglu = din(nc, "wglu", [D, 2048])
    convw = din(nc, "convw", [128, 8 * 31])
    cvec = din(nc, "cvec", [128, 24])
    cm = dout(nc, "cmT", [1024, T], BF16)
    P = Prog(nc)
    C = setup_consts(P)
    V_, G_ = "vector", "gpsimd"
    cwt = P.sb("cwt", [128, 8, 31], F32)
    P.dma("sync", cwt[:], convw.rearrange("p (c j) -> p c j", j=31), writes=["cwt"])
    cvt = P.sb("cvt", [128, 24], F32)
    P.dma("sync", cvt[:], cvec, writes=["cvt"])
    cT = P.sb("cT", [128, 8, HAL + T], BF16)
    xv = xTh.rearrange("(kc p) t -> p kc t", p=128)
    chunks = [(0, HAL)] + [(HAL + i * 512, 512) for i in range(T // 512)]
    with P.scope():
        w_bf, wks = load_w_bf(P, "wglu_bf", wglu, D, 2048)
        xcr = Rot(P, "xTc", [128, 16, 512], BF16, 2)
        sgr = Rot(P, "sig", [128, 512], F32, 2)
        pa_r = Rot(P, "psa", [128, 512], F32, 4, psum=True)
        pg_r = Rot(P, "psg", [128, 512], F32, 4, psum=True)
        for ci, (c0, n) in enumerate(chunks):
            xc, xk = xcr.next()
            for q4 in range(4):
                P.dma("gpsimd", xc[:, q4 * 4:(q4 + 1) * 4, 0:n], xv[:, q4 * 4:(q4 + 1) * 4, c0:c0 + n], writes=[(xk, q4)])
            xks = [(xk, q4) for q4 in range(4)]
            for cc in range(8):
                pa, pak = pa_r.next()
                for kc in range(16):
                    P.mm(pa[:, 0:n], w_bf[:, kc, cc * 128:(cc + 1) * 128], xc[:, kc, 0:n], start=(kc == 0), stop=(kc == 15), reads=wks + xks, writes=[pak])
                pg, pgk = pg_r.next()
                for kc in range(16):
                    P.mm(pg[:, 0:n], w_bf[:, kc, 1024 + cc * 128:1024 + (cc + 1) * 128], xc[:, kc, 0:n], start=(kc == 0), stop=(kc == 15), reads=wks + xks, writes=[pgk])
                sg, sgk = sgr.next()
                P.act(sg[:, 0:n], pg[:, 0:n], AF.Sigmoid, reads=[pgk], writes=[sgk])
                P.tt(V_, cT[:, cc, c0:c0 + n], pa[:, 0:n], sg[:, 0:n], ALU.mult, reads=[pak, sgk], writes=[("cT", cc, ci)])
    with P.scope():
        dg = P.sb("diag", [128, 8 * 31, 128], BF16)
        for cc in range(8):
            for j in range(31):
                P.ts(V_ if (j % 2 == 0) else G_, dg[:, cc * 31 + j, :], C["ident_b"][:], cwt[:, cc, j:j + 1], None, ALU.mult,
                     reads=["ident_b", "cwt"], writes=[("dg", cc, j)])
        convs = P.sb("convs", [128, 8, 512], F32)
        sqr = Rot(P, "sq", [128, 512], F32, 2)
        mean = P.sb("mean", [128, 512], F32)
        msq = P.sb("msq", [128, 512], F32)
        rstd = P.sb("rstd", [128, 512], F32)
        tmr = Rot(P, "tm", [128, 512], F32, 2)
        obr = Rot(P, "ob", [128, 8, 512], BF16, 2)
        pcr = Rot(P, "psc", [128, 512], F32, 4, psum=True)
        ps1r = Rot(P, "ps1", [128, 512], F32, 2, psum=True)
        ps2r = Rot(P, "ps2", [128, 512], F32, 2, psum=True)
        cmv = cm.rearrange("(c p) t -> p c t", p=128)
        for tc in range(T // 512):
            t0 = HAL + tc * 512
            cidx = [ci for ci, (c0, n) in enumerate(chunks) if c0 < t0 + 512 and c0 + n > t0 - 30]
            s1, s1k = ps1r.next()
            s2, s2k = ps2r.next()
            for cc in range(8):
                pc, pck = pcr.next()
                for j in range(31):
                    P.mm(pc[:, :], dg[:, cc * 31 + j, :], cT[:, cc, t0 - 30 + j:t0 - 30 + j + 512], start=(j == 0), stop=(j == 30),
                         reads=[("dg", cc, j)] + [("cT", cc, ci) for ci in cidx], writes=[pck])
                P.act(convs[:, cc, :], pc[:, :], AF.Identity, bias=cvt[:, cc:cc + 1], scale=1.0, reads=[pck, "cvt"], writes=[("convs", cc)])
                sq, sqk = sqr.next()
                P.tt(G_, sq[:], convs[:, cc, :], convs[:, cc, :], ALU.mult, reads=[("convs", cc)], writes=[sqk])
                P.mm(s1[:, :], C["ones_f"][:], convs[:, cc, :], start=(cc == 0), stop=(cc == 7), reads=["ones_f", ("convs", cc)], writes=[s1k])
                P.mm(s2[:, :], C["ones_f"][:], sq[:], start=(cc == 0), stop=(cc == 7), reads=["ones_f", sqk], writes=[s2k])
            P.ts(V_, mean[:], s1[:, :], float(1.0 / 1024), None, ALU.mult, reads=[s1k], writes=["mean"])
            P.tt(V_, msq[:], mean[:], mean[:], ALU.mult, reads=["mean"], writes=["msq"])
            P.stt(V_, msq[:], s2[:, :], float(1.0 / 1024), msq[:], ALU.mult, ALU.subtract, reads=[s2k, "msq"], writes=["msq"])
            P.act(rstd[:], msq[:], AF.Sqrt, bias=C["eps"][:], scale=1.0, reads=["msq", "eps"], writes=["rstd"])
            P.recip(rstd[:], rstd[:], reads=["rstd"], writes=["rstd"])
            ob, obk = obr.next()
            for cc in range(8):
                tm, tmk = tmr.next()
                P.tt(G_, tm[:], convs[:, cc, :], mean[:], ALU.subtract, reads=[("convs", cc), "mean"], writes=[tmk])
                P.tt(V_, tm[:], tm[:], rstd[:], ALU.mult, reads=[tmk, "rstd"], writes=[tmk])
                P.act(ob[:, cc, :], tm[:], AF.Silu, scale=cvt[:, 8 + cc:9 + cc], bias=cvt[:, 16 + cc:17 + cc], reads=[tmk, "cvt"], writes=[(obk, cc)])
            P.dma("sync", cmv[:, :, tc * 512:(tc + 1) * 512], ob[:], reads=[(obk, cc) for cc in range(8)], writes=[("cm", tc)])
    P.build()
    return nc


def build_sgu(T):
    nc = new_nc()
    xT = din(nc, "xT", [D, T], BF16)
    wuv = din(nc, "wuv", [D, 2048])
    lg_ = din(nc, "lng", [1, 1024])
    lb_ = din(nc, "lnb", [1, 1024])
    swT = din(nc, "swT", [128, 4 * 128])
    sgb = din(nc, "sgb", [1, 512])
    sp = dout(nc, "spT", [1024, T], BF16)
    P = Prog(nc)
    C = setup_consts(P)
    V_, G_ = "vector", "gpsimd"
    ln = LN(P, C, 1024, lg_, lb_, "sln")
    wf = P.sb("swf", [128, 4, 128], F32)
    P.dma("sync", wf[:], swT.rearrange("p (g t) -> p g t", t=128), writes=["swf"])
    for gi in range(4):
        P.op(G_, lambda e, gi=gi: e.affine_select(wf[:, gi, :], wf[:, gi, :], [[1, 128]], ALU.is_ge, 0.0, base=0, channel_multiplier=-1),
             reads=["swf"], writes=["swf"])
    wc = P.sb("swc", [128, 4, 128], BF16)
    P.copy(V_, wc[:], wf[:], reads=["swf"], writes=["swc"])
    bT = load_bcast(P, "sbT", sgb, 512)
    bT8 = P.sb("bT8", [128, 8, 128], F32)
    for cc in range(8):
        P.copy(V_, bT8[:, cc, :], bT[:, (cc // 2) * 128:(cc // 2 + 1) * 128], reads=["sbT"], writes=[("bT8", cc)])
    b8k = [("bT8", cc) for cc in range(8)]
    w_bf, wks = load_w_bf(P, "wuv_bf", wuv, D, 2048)
    xcr = Rot(P, "xTc", [128, 16, 512], BF16, 2)
    uT = P.sb("uT", [128, 8, 512], F32)
    vzr = Rot(P, "vz", [128, 1024], F32, 3)
    vgr = Rot(P, "vg", [128, 1024], BF16, 4)
    tmr = Rot(P, "stm", [128, 4, 128], F32, 2)
    spr = Rot(P, "spo", [128, 8, 512], BF16, 2)
    pur = Rot(P, "psu", [128, 512], F32, 2, psum=True)
    pvr = Rot(P, "psv", [128, 512], F32, 2, psum=True)
    psr = Rot(P, "pss", [128, 512], F32, 4, psum=True)
    xv = xT.rearrange("(kc p) t -> p kc t", p=128)
    spv = sp.rearrange("(c p) t -> p c t", p=128)
    for tc in range(T // 512):
        xc, xk = xcr.next()
        for q4 in range(4):
            P.dma("sync", xc[:, q4 * 4:(q4 + 1) * 4, :], xv[:, q4 * 4:(q4 + 1) * 4, tc * 512:(tc + 1) * 512], writes=[(xk, q4)])
        xks = [(xk, q4) for q4 in range(4)]
        for cc in range(8):
            pu, puk = pur.next()
            for kc in range(16):
                P.mm(pu[:, :], w_bf[:, kc, cc * 128:(cc + 1) * 128], xc[:, kc, :], start=(kc == 0), stop=(kc == 15), reads=wks + xks, writes=[puk])
            P.act(uT[:, cc, :], pu[:, :], AF.Gelu, reads=[puk], writes=[("uT", cc)])
        so, sok = spr.next()

        def emit_v(tt, xc=xc, xks=xks):
            vz, vzk = vzr.next()
            for hf in range(2):
                pv, pvk = pvr.next()
                for kc in range(16):
                    P.mm(pv[:, :], xc[:, kc, tt * 128:(tt + 1) * 128], w_bf[:, kc, 1024 + hf * 512:1024 + (hf + 1) * 512], start=(kc == 0), stop=(kc == 15),
                         reads=wks + xks, writes=[pvk])
                P.act(vz[:, hf * 512:(hf + 1) * 512], pv[:, :], AF.Gelu, reads=[pvk], writes=[(vzk, hf)])
            vzks = [(vzk, 0), (vzk, 1)]
            _ln_emit_multi(P, ln, vz, vzks)
            vg, vgk = vgr.next()
            P.copy("scalar", vg[:], vz[:], reads=vzks, writes=[vgk])
            return vg, vgk

        def emit_sv(tt, vg, vgk, so=so, sok=sok):
            for hb in range(2):
                pb, pbk = psr.next()
                for c4 in range(4):
                    cc = hb * 4 + c4
                    P.mm(pb[:, c4 * 128:(c4 + 1) * 128], vg[:, cc * 128:(cc + 1) * 128], wc[:, cc // 2, :], reads=[vgk, "swc"], writes=[pbk])
                tm, tmk = tmr.next()
                P.tt(V_, tm[:], pb[:, :].rearrange("p (c t) -> p c t", t=128), bT8[:, hb * 4:(hb + 1) * 4, :], ALU.add, reads=[pbk] + b8k, writes=[tmk])
                P.tt(V_, so[:, hb * 4:(hb + 1) * 4, tt * 128:(tt + 1) * 128], tm[:], uT[:, hb * 4:(hb + 1) * 4, tt * 128:(tt + 1) * 128], ALU.mult,
                     reads=[tmk] + [("uT", hb * 4 + c4) for c4 in range(4)], writes=[(sok, tt, hb)])

        vgs = [emit_v(0), emit_v(1)]
        for tt in range(4):
            if tt + 2 < 4:
                vgs.append(emit_v(tt + 2))
            emit_sv(tt, *vgs[tt])
        P.dma("sync", spv[:, :, tc * 512:(tc + 1) * 512], so[:], reads=[(sok, tt, hb) for tt in range(4) for hb in range(2)], writes=[("sp", tc)])
    P.build()
    return nc


def build_sconv(T):
    nc = new_nc()
    xTh = din(nc, "xTh", [D, 2 + T], BF16)
    wsc = din(nc, "wsc", [D, 3072])
    scw = din(nc, "scw", [128, 24])
    cv = dout(nc, "cvT", [1024, T], BF16)
    P = Prog(nc)
    C = setup_consts(P)
    V_, G_ = "vector", "gpsimd"
    swt = P.sb("scwt", [128, 8, 3], F32)
    P.dma("sync", swt[:], scw.rearrange("p (c j) -> p c j", j=3), writes=["scwt"])
    w_bf, wks = load_w_bf(P, "wsc_bf", wsc, D, 3072)
    xcr = Rot(P, "xTc", [128, 16, 512], BF16, 2)
    prod = P.sb("prod", [128, 8, 514], F32)
    gcr = Rot(P, "gcs", [128, 512], F32, 2)
    acr = Rot(P, "acc", [128, 512], F32, 2)
    obr = Rot(P, "ob", [128, 8, 512], BF16, 2)
    pbr = Rot(P, "psb", [128, 512], F32, 2, psum=True)
    pcr = Rot(P, "psc", [128, 512], F32, 3, psum=True)
    pxr = Rot(P, "psx", [128, 512], F32, 3, psum=True)
    xv = xTh.rearrange("(kc p) t -> p kc t", p=128)
    cvv = cv.rearrange("(c p) t -> p c t", p=128)
    chunks = [(0, 2, -1)] + [(2 + i * 512, 512, i) for i in range(T // 512)]
    for (c0, n, tc) in chunks:
        xc, xk = xcr.next()
        for q4 in range(4):
            P.dma("sync", xc[:, q4 * 4:(q4 + 1) * 4, 0:n], xv[:, q4 * 4:(q4 + 1) * 4, c0:c0 + n], writes=[(xk, q4)])
        xks = [(xk, q4) for q4 in range(4)]
        if tc >= 0:
            ob, obk = obr.next()
        for cc in range(8):
            pc, pck = pcr.next()
            for kc in range(16):
                P.mm(pc[:, 0:n], w_bf[:, kc, 1024 + cc * 128:1024 + (cc + 1) * 128], xc[:, kc, 0:n], start=(kc == 0), stop=(kc == 15), reads=wks + xks, writes=[pck])
            px, pxk = pxr.next()
            for kc in range(16):
                P.mm(px[:, 0:n], w_bf[:, kc, 2048 + cc * 128:2048 + (cc + 1) * 128], xc[:, kc, 0:n], start=(kc == 0), stop=(kc == 15), reads=wks + xks, writes=[pxk])
            gs, gsk = gcr.next()
            P.copy("scalar", gs[:, 0:n], pc[:, 0:n], reads=[pck], writes=[gsk])
            pk_ = ("prod", cc)
            if tc < 0:
                P.tt(V_, prod[:, cc, 0:2], gs[:, 0:2], px[:, 0:2], ALU.mult, reads=[gsk, pxk], writes=[pk_])
                continue
            pb, pbk = pbr.next()
            for kc in range(16):
                P.mm(pb[:, :], w_bf[:, kc, cc * 128:(cc + 1) * 128], xc[:, kc, :], start=(kc == 0), stop=(kc == 15), reads=wks + xks, writes=[pbk])
            P.tt(V_, prod[:, cc, 2:514], gs[:], px[:, :], ALU.mult, reads=[gsk, pxk, pk_], writes=[pk_])
            ac, ack = acr.next()
            P.act(ac[:], prod[:, cc, 2:514], AF.Identity, scale=swt[:, cc, 2:3], reads=[pk_, "scwt"], writes=[ack])
            P.stt(V_, ac[:], prod[:, cc, 1:513], swt[:, cc, 1:2], ac[:], ALU.mult, ALU.add, reads=[pk_, "scwt", ack], writes=[ack])
            P.stt(V_, ac[:], prod[:, cc, 0:512], swt[:, cc, 0:1], ac[:], ALU.mult, ALU.add, reads=[pk_, "scwt", ack], writes=[ack])
            P.tt(V_, ob[:, cc, :], ac[:], pb[:, :], ALU.mult, reads=[ack, pbk], writes=[(obk, cc)])
            P.copy(G_, prod[:, cc, 0:2], prod[:, cc, 512:514], reads=[pk_], writes=[pk_])
        if tc >= 0:
            P.dma("sync", cvv[:, :, tc * 512:(tc + 1) * 512], ob[:], reads=[(obk, cc) for cc in range(8)], writes=[("cv", tc)])
    P.build()
    return nc


B_, S_, T_ = 2, 16384, 4096
NCORE = 8


def _rope_consts():
    inv = (500000.0 ** (-np.arange(0, 32, 2, dtype=np.float32) / 32)).astype(np.float32)
    PI = 3.1415925
    rc = np.zeros((32, 5), np.float32)
    for p in range(32):
        s = -1.0 if p < 16 else 1.0
        rc[p] = [inv[p % 16] / (2 * np.pi), s * 2 * PI, 2 * PI, 0, 0]
    return rc


def _perm_cols(w):
    return np.ascontiguousarray(np.concatenate(
        [np.concatenate([w[:, m * 128 + 16:m * 128 + 32], w[:, m * 128:m * 128 + 16]], 1) for m in range(2)], 1))


def _pl(v, n):
    return np.ascontiguousarray(np.asarray(v).reshape(n, 128).T)


def _run(nc, in_maps):
    res = run_bass_kernel_spmd(nc, in_maps, core_ids=list(range(NCORE)))
    return res.results


def _c(a):
    return np.ascontiguousarray(a)


def kernel(**inp):
    f32 = np.float32
    x = np.asarray(inp["x"], f32)
    mem = np.asarray(inp["mem"], f32)
    positions = np.asarray(inp["positions"], np.int32)
    w_in = np.asarray(inp["w_in"], f32)
    w_out = np.asarray(inp["w_out"], f32)
    cores = [(c // 4, c % 4) for c in range(NCORE)]

    xTb = [_c(x[b].T) for b in range(B_)]
    w0 = w_in[0]
    lamv = _c(np.concatenate([inp["lam_q1"][0], inp["lam_k1"][0], inp["lam_q2"][0], inp["lam_k2"][0]])[None].astype(f32))
    subg = _pl(inp["diff_subln_g"][0].astype(f32), 2)
    rc = _rope_consts()
    maps = []
    for (b, h) in cores:
        wq = _c(w0[:, h * 256:(h + 1) * 256])
        wk = _c(w0[:, 1024 + h * 256:1024 + (h + 1) * 256])
        wv = _c(w0[:, 2048 + h * 256:2048 + (h + 1) * 256])
        maps.append(dict(xT=xTb[b], wq=wq, wk=wk, wv=wv, wqp=_perm_cols(wq), wkp=_perm_cols(wk), pos=_c(positions[b][None]),
                         rc=rc, lamv=lamv, subg=subg))
    r = _run(build_attn(S_), maps)
    oT = [[r[b * 4 + h]["oT"] for h in range(4)] for b in range(B_)]

    cw = np.asarray(inp["conv_w"][0], f32)
    convw = _c(cw.T.reshape(8, 128, 31).transpose(1, 0, 2).reshape(128, 8 * 31))
    cvec = _c(np.concatenate([_pl(inp["conv_b"][0].astype(f32), 8), _pl(inp["conv_ln_g"][0].astype(f32), 8), _pl(inp["conv_ln_b"][0].astype(f32), 8)], 1))
    wglu = _c(w0[:, 3072:5120])
    maps = []
    for (b, rr) in cores:
        xh = np.zeros((D, 128 + T_), f32)
        lo = rr * T_ - 128
        if lo >= 0:
            xh[:] = xTb[b][:, lo:(rr + 1) * T_]
        else:
            xh[:, 128:] = xTb[b][:, 0:T_]
        maps.append(dict(xTh=xh, wglu=wglu, convw=convw, cvec=cvec))
    r = _run(build_conf(T_), maps)
    cmT = [r[c]["cmT"] for c in range(NCORE)]

    nc_out = build_outproj(T_)
    nc_cross = build_cross(T_)
    nc_moe = build_moe(T_)
    memT = [_c(mem[b].T) for b in range(B_)]
    kvw = np.asarray(inp["mem_kv_w"], f32)
    ecap = (np.arange(32, dtype=f32) * CAP)[None]

    def tail(l, mixT, xcur):
        maps = [dict(mixT=mixT[c], xres=xcur[c], w=_c(w_out[l]), g=_c(inp["ln_mix_g"][l][None].astype(f32)), b=_c(inp["ln_mix_b"][l][None].astype(f32)))
                for c in range(NCORE)]
        r = _run(nc_out, maps)
        x1 = [r[c]["y"] for c in range(NCORE)]
        x1b = [r[c]["yb"] for c in range(NCORE)]
        maps = [dict(xT=_c(x1b[c].T), xres=x1[c], memT=memT[cores[c][0]], kvw=kvw, xqw=_c(inp["xq_w"][l].astype(f32)), xow=_c(inp["xo_w"][l].astype(f32)),
                     g=_c(inp["ln_mem_g"][l][None].astype(f32)), b=_c(inp["ln_mem_b"][l][None].astype(f32))) for c in range(NCORE)]
        r = _run(nc_cross, maps)
        x2 = [r[c]["y"] for c in range(NCORE)]
        rw = _c(np.concatenate([inp["rg_w"][l], inp["re_w"][l]], 1).astype(f32))
        rb = _c(np.concatenate([inp["rg_b"][l], inp["re_b"][l]])[None].astype(f32))
        w1, w3, w2 = _c(inp["e_w1"][l].astype(f32)), _c(inp["e_w3"][l].astype(f32)), _c(inp["e_w2"][l].astype(f32))
        maps = [dict(xT=_c(x2[c].T), xres=x2[c], rw=rw, rb=rb, ecap=ecap, w1=w1, w3=w3, w2=w2,
                     g=_c(inp["ln_ffn_g"][l][None].astype(f32)), b=_c(inp["ln_ffn_b"][l][None].astype(f32))) for c in range(NCORE)]
        r = _run(nc_moe, maps)
        print("[moe] layer", l, "max per-core per-expert load:", max(float(r[c]["cnt"].max()) for c in range(NCORE)), "capacity", CAP, flush=True)
        return [r[c]["y"] for c in range(NCORE)], [r[c]["yb"] for c in range(NCORE)]

    mix0 = [_c(np.concatenate([oT[b][h][:, rr * T_:(rr + 1) * T_] for h in range(4)] + [cmT[c]], 0)) for c, (b, rr) in enumerate(cores)]
    xcur = [_c(x[b, rr * T_:(rr + 1) * T_]) for (b, rr) in cores]
    xl0, xl0b = tail(0, mix0, xcur)

    w1_ = w_in[1]
    sw = np.asarray(inp["sgu_w"][0], f32)
    swT = _c(sw.transpose(2, 0, 1).reshape(128, 512))
    sgb = _c(np.asarray(inp["sgu_b"][0], f32).reshape(1, 512))
    xl0T = [_c(a.T) for a in xl0b]
    maps = [dict(xT=xl0T[c], wuv=_c(w1_[:, 0:2048]), lng=_c(inp["sgu_ln_g"][0][None].astype(f32)), lnb=_c(inp["sgu_ln_b"][0][None].astype(f32)),
                 swT=swT, sgb=sgb) for c in range(NCORE)]
    r = _run(build_sgu(T_), maps)
    spT = [r[c]["spT"] for c in range(NCORE)]
    scw = _c(np.asarray(inp["sc_w"][0], f32).T.reshape(8, 128, 3).transpose(1, 0, 2).reshape(128, 24))
    maps = []
    for c, (b, rr) in enumerate(cores):
        xh = np.zeros((D, 2 + T_), xl0T[c].dtype)
        xh[:, 2:] = xl0T[c]
        if rr > 0:
            xh[:, 0:2] = xl0T[c - 1][:, T_ - 2:T_]
        maps.append(dict(xTh=xh, wsc=_c(w1_[:, 2048:5120]), scw=scw))
    r = _run(build_sconv(T_), maps)
    cvT = [r[c]["cvT"] for c in range(NCORE)]
    mix1 = [_c(np.concatenate([spT[c], cvT[c]], 0)) for c in range(NCORE)]
    xl1, _unused = tail(1, mix1, xl0)
    out = np.zeros((B_, S_, D), f32)
    for c, (b, rr) in enumerate(cores):
        out[b, rr * T_:(rr + 1) * T_] = xl1[c]
    return out
```
